# Optimizing a Trainium2 kernel written in Bass

```python
import jax, jax.numpy as jnp
from jax import lax
import numpy as np

D_MODEL = 2048
BATCH = 2
SEQ = 4096
DEPTH = 1

GM_WIDTH = D_MODEL // 2
GM_GROUP_DIM = 128
GM_GROUPS = GM_WIDTH // GM_GROUP_DIM
GM_CHUNK = 128
ML_HEADS = 4
ML_HEAD_DIM = (D_MODEL // 2) // ML_HEADS
ML_WIDTH = ML_HEADS * ML_HEAD_DIM
ML_CHUNK = 128
CONV_K = 4
D_FF = 4 * D_MODEL
EPS = 1e-6
IN_WIDTHS = (GM_WIDTH, GM_WIDTH, ML_WIDTH, ML_WIDTH, ML_WIDTH, ML_HEADS, ML_HEADS, D_MODEL, D_MODEL)
N_IN = 2 * GM_WIDTH + 3 * ML_WIDTH + 2 * ML_HEADS + 2 * D_MODEL

kernel_name = "hybrid_gmlp_mlstm_gated_block"


def _rms_norm(x, g):
    xf = x.astype(jnp.float32)
    y = xf * lax.rsqrt(jnp.mean(xf * xf, axis=-1, keepdims=True) + EPS)
    return (y * g.astype(jnp.float32)).astype(x.dtype)


def _layer_norm(x, g, b):
    xf = x.astype(jnp.float32)
    mu = jnp.mean(xf, axis=-1, keepdims=True)
    var = jnp.mean(jnp.square(xf - mu), axis=-1, keepdims=True)
    y = (xf - mu) * lax.rsqrt(var + EPS)
    return (y * g.astype(jnp.float32) + b.astype(jnp.float32)).astype(x.dtype)


def _gmlp_mixer(u, v, ln_g, ln_b, ws, bs):
    B, S, _ = u.shape
    u = jax.nn.gelu(u)
    v = _layer_norm(jax.nn.gelu(v), ln_g, ln_b)
    nc = S // GM_CHUNK
    vc = v.reshape(B, nc, GM_CHUNK, GM_GROUPS, GM_GROUP_DIM)
    causal = jnp.tril(jnp.ones((GM_CHUNK, GM_CHUNK), dtype=bool))
    ws_c = jnp.where(causal[None], ws, jnp.zeros_like(ws))
    s = jnp.einsum('gts,bnsgc->bntgc', ws_c, vc) + bs.T[:, :, None]
    return u * s.reshape(B, S, GM_WIDTH)


def _causal_conv(x, w, b):
    S = x.shape[1]
    xp = jnp.pad(x, ((0, 0), (CONV_K - 1, 0), (0, 0)))
    out = b
    for j in range(CONV_K):
        out = out + w[j] * xp[:, j:j + S]
    return out


def _mlstm_mixer(m_in, v_in, o_in, i_pre, f_pre, conv_w, conv_b, wq, wk, ig_b, fg_b, norm_g):
    B, S, _ = m_in.shape
    H, Dh, L = ML_HEADS, ML_HEAD_DIM, ML_CHUNK
    nc = S // L
    f32 = jnp.float32
    c = jax.nn.silu(_causal_conv(m_in, conv_w, conv_b)).reshape(B, S, H, Dh)
    q = jnp.einsum('bshd,hde->bhse', c, wq).astype(f32)
    k = (jnp.einsum('bshd,hde->bhse', c, wk) * (Dh ** -0.5)).astype(f32)
    v = v_in.reshape(B, S, H, Dh).transpose(0, 2, 1, 3).astype(f32)
    ig = (i_pre + ig_b).astype(f32).transpose(0, 2, 1)
    logf = jax.nn.log_sigmoid((f_pre + fg_b).astype(f32)).transpose(0, 2, 1)
    q = q.reshape(B, H, nc, L, Dh)
    k = k.reshape(B, H, nc, L, Dh)
    v = v.reshape(B, H, nc, L, Dh)
    ig = ig.reshape(B, H, nc, L)
    bcum = jnp.cumsum(logf.reshape(B, H, nc, L), axis=-1)
    b_last = bcum[..., -1]
    a = b_last[..., None] - bcum + ig
    a_max = jnp.max(a, axis=-1)
    wa = jnp.exp(a - a_max[..., None])
    kv = jnp.einsum('bhnl,bhnld,bhnle->bhnde', wa, k, v)
    ksum = jnp.einsum('bhnl,bhnld->bhnd', wa, k)

    def step(carry, inp):
        C, n, m = carry
        kv_c, ks_c, bl_c, am_c = inp
        m_new = jnp.maximum(bl_c + m, am_c)
        decay = jnp.exp(bl_c + m - m_new)
        scale = jnp.exp(am_c - m_new)
        C_new = decay[..., None, None] * C + scale[..., None, None] * kv_c
        n_new = decay[..., None] * n + scale[..., None] * ks_c
        return (C_new, n_new, m_new), (C, n, m)

    init = (jnp.zeros((B, H, Dh, Dh), f32), jnp.zeros((B, H, Dh), f32), jnp.zeros((B, H), f32))
    xs = (jnp.moveaxis(kv, 2, 0), jnp.moveaxis(ksum, 2, 0), jnp.moveaxis(b_last, 2, 0), jnp.moveaxis(a_max, 2, 0))
    _, (C_prev, n_prev, m_prev) = lax.scan(step, init, xs)
    C_prev = jnp.moveaxis(C_prev, 0, 2)
    n_prev = jnp.moveaxis(n_prev, 0, 2)
    m_prev = jnp.moveaxis(m_prev, 0, 2)

    causal = jnp.tril(jnp.ones((L, L), dtype=bool))
    Dlog = bcum[..., :, None] - bcum[..., None, :] + ig[..., None, :]
    Dlog = jnp.where(causal, Dlog, -jnp.inf)
    inter = bcum + m_prev[..., None]
    m_t = jnp.maximum(inter, jnp.max(Dlog, axis=-1))
    w_intra = jnp.exp(Dlog - m_t[..., None])
    w_inter = jnp.exp(inter - m_t)
    qk = jnp.einsum('bhntd,bhnsd->bhnts', q, k) * w_intra
    num = (w_inter[..., None] * jnp.einsum('bhntd,bhnde->bhnte', q, C_prev)
           + jnp.einsum('bhnts,bhnse->bhnte', qk, v))
    den = w_inter * jnp.einsum('bhntd,bhnd->bhnt', q, n_prev) + jnp.sum(qk, axis=-1)
    h = num / jnp.maximum(jnp.abs(den), jnp.exp(-m_t))[..., None]
    h = h.reshape(B, H, S, Dh).transpose(0, 2, 1, 3)
    mu = jnp.mean(h, axis=-1, keepdims=True)
    var = jnp.mean(jnp.square(h - mu), axis=-1, keepdims=True)
    h = ((h - mu) * lax.rsqrt(var + EPS)).reshape(B, S, ML_WIDTH) * norm_g.astype(f32)
    return (jax.nn.sigmoid(o_in.astype(f32)) * h).astype(m_in.dtype)


def setup_inputs(seed: int = 0) -> dict:
    key = jax.random.key(seed)
    ks = jax.random.split(key, 24)
    nrm = lambda k, shape, s: jax.random.normal(k, shape, jnp.float32) * s
    Ld = DEPTH
    return {
        "x": jax.random.normal(ks[0], (BATCH, SEQ, D_MODEL), jnp.float32),
        "norm1_g": 1.0 + nrm(ks[1], (Ld, D_MODEL), 0.02),
        "w_in": nrm(ks[2], (Ld, D_MODEL, N_IN), D_MODEL ** -0.5),
        "b_gate": nrm(ks[3], (Ld, 2, D_MODEL), 0.02),
        "gm_ln_g": 1.0 + nrm(ks[4], (Ld, GM_WIDTH), 0.02),
        "gm_ln_b": nrm(ks[5], (Ld, GM_WIDTH), 0.02),
        "gm_ws": nrm(ks[6], (Ld, GM_GROUPS, GM_CHUNK, GM_CHUNK), GM_CHUNK ** -0.5),
        "gm_bs": 1.0 + nrm(ks[7], (Ld, GM_GROUPS, GM_CHUNK), 0.02),
        "ml_conv_w": nrm(ks[8], (Ld, CONV_K, ML_WIDTH), CONV_K ** -0.5),
        "ml_conv_b": nrm(ks[9], (Ld, ML_WIDTH), 0.02),
        "ml_wq": nrm(ks[10], (Ld, ML_HEADS, ML_HEAD_DIM, ML_HEAD_DIM), ML_HEAD_DIM ** -0.5),
        "ml_wk": nrm(ks[11], (Ld, ML_HEADS, ML_HEAD_DIM, ML_HEAD_DIM), ML_HEAD_DIM ** -0.5),
        "ml_ig_b": nrm(ks[12], (Ld, ML_HEADS), 0.1),
        "ml_fg_b": jnp.linspace(3.0, 6.0, ML_HEADS, dtype=jnp.float32)[None] + nrm(ks[13], (Ld, ML_HEADS), 0.1),
        "ml_norm_g": 1.0 + nrm(ks[14], (Ld, ML_WIDTH), 0.02),
        "w_a": nrm(ks[15], (Ld, GM_WIDTH, D_MODEL), GM_WIDTH ** -0.5),
        "w_b": nrm(ks[16], (Ld, ML_WIDTH, D_MODEL), ML_WIDTH ** -0.5),
        "w_out": nrm(ks[17], (Ld, D_MODEL, D_MODEL), D_MODEL ** -0.5),
        "norm2_g": 1.0 + nrm(ks[18], (Ld, D_MODEL), 0.02),
        "w_ff1": nrm(ks[19], (Ld, D_MODEL, D_FF), D_MODEL ** -0.5),
        "w_ff2": nrm(ks[20], (Ld, D_FF, D_MODEL), D_FF ** -0.5),
        "norm_f_g": 1.0 + nrm(ks[21], (D_MODEL,), 0.02),
    }


def reference(x, norm1_g, w_in, b_gate, gm_ln_g, gm_ln_b, gm_ws, gm_bs, ml_conv_w, ml_conv_b,
              ml_wq, ml_wk, ml_ig_b, ml_fg_b, ml_norm_g, w_a, w_b, w_out, norm2_g, w_ff1, w_ff2,
              norm_f_g):
    split_idx = [int(i) for i in np.cumsum(IN_WIDTHS)[:-1]]
    for l in range(DEPTH):
        xn = _rms_norm(x, norm1_g[l])
        proj = xn @ w_in[l]
        gm_u, gm_v, ml_m, ml_v, ml_o, ml_i, ml_f, g_a, g_b = jnp.split(proj, split_idx, axis=-1)
        y_a = _gmlp_mixer(gm_u, gm_v, gm_ln_g[l], gm_ln_b[l], gm_ws[l], gm_bs[l])
        y_b = _mlstm_mixer(ml_m, ml_v, ml_o, ml_i, ml_f, ml_conv_w[l], ml_conv_b[l], ml_wq[l],
                           ml_wk[l], ml_ig_b[l], ml_fg_b[l], ml_norm_g[l])
        gate_a = jax.nn.sigmoid(g_a + b_gate[l, 0])
        gate_b = jax.nn.sigmoid(g_b + b_gate[l, 1])
        mixed = gate_a * (y_a @ w_a[l]) + gate_b * (y_b @ w_b[l])
        x = x + mixed @ w_out[l]
        hn = _rms_norm(x, norm2_g[l])
        x = x + jnp.square(jax.nn.relu(hn @ w_ff1[l])) @ w_ff2[l]
    return _rms_norm(x, norm_f_g)
```

```python
import contextlib
import numpy as np
import concourse.bass as bass
import concourse.mybir as mybir
from concourse.bass_utils import run_bass_kernel_spmd

F32 = mybir.dt.float32
BF16 = mybir.dt.bfloat16
ALU = mybir.AluOpType
AF = mybir.ActivationFunctionType
AX = mybir.AxisListType
EPS = 1e-6
NEG = -1.0e30

REAL_CFG = dict(D=2048, T=1024, NPREV=3072, H=4, FG=2048, NCORES=8)


class Sched:
    def __init__(self):
        self.ops = []
        self.lastw = {}
        self.readers = {}

    def op(self, eng, fn, reads=(), writes=(), dma=None):
        idx = len(self.ops)
        deps = set()
        for k in reads:
            if k in self.lastw:
                deps.add(self.lastw[k])
        for k in writes:
            if k in self.lastw:
                deps.add(self.lastw[k])
            for r in self.readers.get(k, ()):
                deps.add(r)
        deps.discard(idx)
        self.ops.append(dict(eng=eng, fn=fn, deps=deps, dma=dma, marked=False, sig=None))
        for k in reads:
            self.readers.setdefault(k, []).append(idx)
        for k in writes:
            self.lastw[k] = idx
            self.readers[k] = []
        return idx

    def finalize(self):
        ops = self.ops
        dcnt = {}
        for o in ops:
            if o['dma']:
                dcnt[o['dma']] = dcnt.get(o['dma'], 0) + 16
                o['sig'] = (('dma', o['dma']), dcnt[o['dma']])
        for o in ops:
            for d in o['deps']:
                p = ops[d]
                if p['dma']:
                    continue
                if o['eng'] == 'pe' and p['eng'] == 'pe' and not o['dma']:
                    continue
                p['marked'] = True
        cnt = {}
        for o in ops:
            if not o['dma'] and o['marked']:
                cnt[o['eng']] = cnt.get(o['eng'], 0) + 1
                o['sig'] = (('eng', o['eng']), cnt[o['eng']])
        return sorted(dcnt.keys())

    def emit_engine(self, eng, handle, sems):
        ops = self.ops
        seen = {}
        for o in ops:
            if o['eng'] != eng:
                continue
            waits = {}
            for d in o['deps']:
                p = ops[d]
                if (not p['dma']) and eng == 'pe' and p['eng'] == 'pe' and not o['dma']:
                    continue
                sk, val = p['sig']
                if waits.get(sk, 0) < val:
                    waits[sk] = val
            for sk, val in waits.items():
                if seen.get(sk, 0) < val:
                    handle.wait_ge(sems[sk], val)
                    seen[sk] = val
            if o['fn'] is None:
                continue
            ins = o['fn'](handle)
            if o['dma']:
                ins.then_inc(sems[('dma', o['dma'])], 16)
            elif o['marked']:
                ins.then_inc(sems[('eng', eng)], 1)


def build(cfg):
    D = cfg['D']; T = cfg['T']; NPREV = cfg['NPREV']; H = cfg['H']; FG = cfg['FG']
    GW = D // 2; G = GW // 128; Dh = GW // H; DC = Dh // 128; KC = D // 128
    MC = GW // 128; DFF = 4 * D; NIN = 5 * GW + 2 * H + 2 * D
    NT = T // 128; BLK = 512; NBO = T // BLK; NBP = NPREV // BLK; NB = NBP + NBO
    NCHT = (NPREV + T) // 128
    HT = T // 2
    NHT = HT // 128
    KF = FG // 128
    FB = min(512, HT)
    NG = DFF // FG
    SL = 8192
    RS = Dh ** -0.5
    c_u, c_v, c_m, c_mv, c_o = 0, GW, 2 * GW, 3 * GW, 4 * GW
    c_i = 5 * GW; c_f = c_i + H; c_ga = c_f + H; c_gb = c_ga + D

    nc = bass.Bass("TRN2", target_bir_lowering=False, dynamic_dma_scratch_size=8192)
    dt_in = lambda n, s: nc.dram_tensor(n, list(s), F32, kind="ExternalInput").ap()
    xseq = dt_in("xseq", [NPREV + T, D])
    w_in = dt_in("w_in", [D, NIN]); w_a = dt_in("w_a", [GW, D]); w_b = dt_in("w_b", [GW, D])
    w_out = dt_in("w_out", [D, D]); w_ff1 = dt_in("w_ff1", [D, DFF]); w_ff2 = dt_in("w_ff2", [DFF, D])
    g1fm = dt_in("g1fm", [128, KC]); g2fm = dt_in("g2fm", [128, KC]); gfbc = dt_in("gfbc", [128, D])
    lngbc = dt_in("lngbc", [128, GW]); lnbbc = dt_in("lnbbc", [128, GW]); bsbc = dt_in("bsbc", [128, GW])
    wsT_d = dt_in("wsT", [128, GW]); tri_d = dt_in("tri", [128, 128])
    cw_d = dt_in("cw", [128, MC * 4]); cb_d = dt_in("cb", [128, MC])
    wq_d = dt_in("wq_s", [128, MC * Dh]); wk_d = dt_in("wk_s", [128, MC * Dh])
    igb_d = dt_in("igb", [H, 1]); fgb_d = dt_in("fgb", [H, 1]); ngbc_d = dt_in("ngbc", [128, GW])
    bgate_d = dt_in("bgate", [128, 2 * KC]); mask_d = dt_in("mask", [H, NCHT])
    ident_d = dt_in("ident", [128, 128]); sel_d = dt_in("sel", [H, H * 128])
    out_d = nc.dram_tensor("out", [T, D], F32, kind="ExternalOutput").ap()

    S = Sched()
    es = contextlib.ExitStack()

    def sb(name, shape, dtype):
        return es.enter_context(nc.sbuf_tensor(name, list(shape), dtype))

    def ps(name, shape, dtype):
        return es.enter_context(nc.psum_tensor(name, list(shape), dtype))

    ROWS_N = 12 * 128
    A_P1 = D + ROWS_N + H * 128 + D // 2 + 2 * 512 + (KC * 512 + MC * 512 + 4 * GW + MC * 512) // 2
    A_N = max(NT * D, A_P1)
    B_N = max(KC * T, NT * GW + 2 * (MC * Dh + 2 * H * 128 + 2 * (Dh + 4)) + H * 128)
    A = sb("A", [128, A_N], F32)
    B = sb("B", [128, B_N], BF16)
    C = sb("C", [128, 16384], BF16)
    RING = sb("RING", [128, 4 * SL], BF16)
    PCN = sb("PCN", [128, 3 * GW], F32)
    A_bf = A[:].bitcast(BF16)
    B_f = B[:].bitcast(F32)
    C_f = C[:].bitcast(F32)

    def vbf(base, off, shape):
        n = int(np.prod(shape))
        v = base[:, off:off + n]
        if len(shape) == 2:
            return v.rearrange("p (a b) -> p a b", a=shape[0])
        if len(shape) == 3:
            return v.rearrange("p (a b c) -> p a b c", a=shape[0], b=shape[1])
        return v

    of = 0
    xst = [A[:, 0:D], A[:, 0:D]]; of = D
    rows = A[0:H, of:of + ROWS_N]; of += ROWS_N
    sel = A[0:H, of:of + H * 128]; of += H * 128
    xs = [A_bf[:, 2 * of:2 * of + D], A_bf[:, 2 * of:2 * of + D]]; of += D // 2
    gblk = A[0:H, of:of + 2 * 512]; of += 2 * 512
    o = 2 * of
    xnTb = vbf(A_bf, o, [KC, BLK]); o += KC * BLK
    cT = vbf(A_bf, o, [MC, BLK]); o += MC * BLK
    kw = vbf(A_bf, o, [4, H, Dh]); o += 4 * GW
    qT = vbf(A_bf, o, [MC, BLK]); o += MC * BLK
    assert o <= 2 * A_N, (o, A_N)
    xnT = vbf(A_bf, 0, [KC, T])
    yaT = vbf(A_bf, KC * T, [G, T])
    ybT = vbf(A_bf, KC * T + G * T, [MC, T])
    x1 = A[:, 0:NHT * D].rearrange("p (a b) -> p a b", a=NHT)
    hnT = vbf(A_bf, 2 * NHT * D, [KC, HT])
    h1T = vbf(A_bf, 2 * NHT * D + KC * HT, [KF, HT])
    assert 2 * NHT * D + KC * HT + KF * HT <= 2 * A_N
    hn_store = vbf(B[:], 0, [NT, GW])
    v_ln = vbf(B[:], NT * GW, [NT, GW])
    mixedT = vbf(B[:], 0, [KC, T])
    ob = NT * GW // 2
    Cst = vbf(B_f, ob, [MC, Dh]); ob += MC * Dh
    Ebuf = vbf(B_f, ob, [H, 128]); ob += H * 128
    zt = vbf(B_f, ob, [H, 128]); ob += H * 128
    numden = B_f[:, ob:ob + Dh + 4]; ob += Dh + 4
    intra_sb = B_f[:, ob:ob + Dh + 4]; ob += Dh + 4
    PT = vbf(B[:], 2 * ob, [H, 128]); ob += H * 64
    assert ob <= B_N // 2, (ob, B_N)
    oc = 0
    VW = Dh + 2
    vsb = vbf(C[:], oc, [4, H, VW]); oc += 4 * H * VW
    kT = vbf(C[:], oc, [MC, BLK]); oc += MC * BLK
    Cb = vbf(C[:], oc, [MC, VW]); oc += MC * VW
    hbuf = C_f[:, oc // 2:oc // 2 + GW]; oc += 2 * GW
    mbuf = [C_f[:, oc // 2 + i * 520:oc // 2 + i * 520 + 520] for i in range(2)]; oc += 2 * 1040
    tacc = C_f[:, oc // 2:oc // 2 + BLK]; oc += 2 * BLK
    assert oc <= 16384, oc
    gx = [C_f[:, i * 512:(i + 1) * 512] for i in range(2)]
    gt = [C_f[:, 1024 + i * 512:1024 + (i + 1) * 512] for i in range(2)]
    gz = [C_f[:, 2048 + i * 512:2048 + (i + 1) * 512] for i in range(2)]
    gv = C_f[:, 3072:3072 + GW]
    tm1 = C_f[:, 3072 + GW:3072 + 2 * GW]
    ybtm = C[:, 2 * (3072 + 2 * GW):2 * (3072 + 2 * GW) + 512]
    assert 3072 + 2 * GW + 256 <= 8192
    gfb = C_f[:, 0:D]
    ost = [C_f[:, D + i * D:D + (i + 1) * D] for i in range(2)]
    assert 3 * D <= 8192 or D < 2048
    wk_s = vbf(PCN[:].bitcast(BF16), 0, [MC, Dh])
    wq_s = vbf(PCN[:].bitcast(BF16), MC * Dh, [MC, Dh])
    XST1_OK = (3 * GW - MC * Dh // 2) >= D and (MC * Dh // 2) <= GW and 2 * GW >= D
    xst1 = PCN[:, 3 * GW - D:3 * GW] if XST1_OK else None
    assert MC * Dh <= 2 * GW
    ngbc = PCN[:, 2 * GW:3 * GW]
    xs1 = hbuf.bitcast(BF16)[:, 0:D] if 2 * GW >= D else None
    lng_s = PCN[:, 0:GW]; lnb_s = PCN[:, GW:2 * GW]; bs_s = PCN[:, 2 * GW:3 * GW]

    g1s = sb("g1s", [128, KC], F32); g2s = sb("g2s", [128, KC], F32)
    wsTm = sb("wsTm", [128, GW], BF16)
    wsT = C[:, 0:GW]
    tri = sb("tri_s", [128, 128], F32)
    cw = sb("cw_s", [128, MC * 4], F32); cb = sb("cb_s", [128, MC], F32)
    igb = sb("igb_s", [H, 1], F32); fgb = sb("fgb_s", [H, 1], F32); nfgb = sb("nfgb", [H, 1], F32)
    bgate = sb("bgate_s", [128, 2 * KC], F32); mask = sb("mask_s", [H, NCHT], F32)
    identb = sb("identb", [128, 128], BF16); identf = sb("identf", [128, 128], F32)
    onesH = sb("onesH", [H, 128], F32)
    onesb = sb("onesb", [128, 1], BF16)
    wg = sb("wg", [128, KC * 2 * H], BF16)
    halo = sb("halo", [128, MC * 4], F32)
    nst = sb("nst", [128, MC], F32)
    stat = sb("stat", [128, 64], F32)
    junkF = C[:, 12288:12288 + D]
    rsm = sb("rsm", [H, 64], F32)
    cols = sb("cols", [128, 4 * H], F32)
    dsb = sb("dsb", [128, 2 * H], F32)
    bnst = sb("bnst", [128, 4 * 6], F32)
    dummy = sb("dummy_t", [128, 4], F32)

    PB = [ps("pb%d" % i, [128, 512], F32) for i in range(8)]

    def pbf(i):
        return PB[i][:].bitcast(BF16)

    REGKEYS = {}

    def K(region, *rest):
        return (region,) + rest

    def op(eng, fn, reads=(), writes=(), dma=None):
        reads = list(reads); writes = list(writes)
        regs = set()
        for k in reads + writes:
            if k[0] in REGKEYS:
                regs.add(('REGION', k[0]))
                REGKEYS[k[0]].add(k)
        return S.op(eng, fn, reads + list(regs), writes, dma)

    for r in ('A', 'B', 'C', 'PCN'):
        REGKEYS[r] = set()

    def region_barrier(region):
        old = list(REGKEYS[region])
        REGKEYS[region] = set()
        S.op('dve', lambda e: e.memset(dummy[:, 0:1], 0.0), reads=(), writes=old + [('REGION', region), ('dummy',)])

    def dma_sp(out, in_, reads, writes, sem):
        op('sp', lambda e: e.dma_start(out=out, in_=in_), reads, writes, dma=sem)

    def dma_cast(out, in_, reads, writes, sem):
        op('pool', lambda e: e.dma_start(out=out, in_=in_), reads, writes, dma=sem)

    CONST = ('consts',)

    for (dst, src) in [(g1s, g1fm), (g2s, g2fm), (tri, tri_d), (cw, cw_d), (cb, cb_d),
                       (igb, igb_d), (fgb, fgb_d), (bgate, bgate_d), (mask, mask_d), (identf, ident_d)]:
        dma_sp(dst[:], src, [], [CONST], 'const')
    dma_cast(identb[:], ident_d, [], [CONST], 'constc')
    dma_cast(wsT, wsT_d, [], [('C', 'vsb')], 'wsld')
    dma_sp(sel, sel_d, [], [('A', 'sel')], 'selc')
    dma_cast(wg[:].rearrange("p (k n) -> p k n", k=KC),
             w_in.rearrange("(k p) n -> p k n", p=128)[:, :, c_i:c_i + 2 * H], [], [CONST], 'constc')

    def init_consts(e):
        e.memset(onesH[:], 1.0)
        e.memset(nhalf[:], -0.5)
        e.memset(onesb[:], 1.0)
        e.memset(halo[:], 0.0)
        e.memset(nst[:], 0.0)
        e.memset(rsm[:], 0.0)
        e.tensor_scalar(out=nfgb[:], in0=fgb[:], scalar1=-1.0, scalar2=None, op0=ALU.mult)
        e.tensor_scalar(out=nbm[:], in0=mask[:], scalar1=-1.0, scalar2=1.0e30, op0=ALU.add, op1=ALU.mult)
        return e.tensor_tensor(out=wsTm[:].rearrange("p (g t) -> p g t", g=G),
                               in0=wsT.rearrange("p (g t) -> p g t", g=G),
                               in1=tri[:].unsqueeze(1).broadcast_to([128, G, 128]), op=ALU.mult)
    op('dve', init_consts, [CONST, ('C', 'vsb')], [('init',)])
    INIT = [CONST, ('init',)]

    ring_state = dict(n=0)

    def slab_load(src_ap, kc, cbw):
        slot = ring_state['n'] % 4
        ring_state['n'] += 1
        view = RING[:, slot * SL:slot * SL + kc * cbw].rearrange("p (k n) -> p k n", k=kc)
        key = ('slab', slot)
        srcv = src_ap.rearrange("(k p) n -> p k n", p=128)
        dma_cast(view, srcv, [], [key], 'slab%d' % slot)
        return view, key, ring_state['n'] - 1

    def mm_fm(bank_ap, slab, col0, act, t0, nt, kc, reads, writes):
        def fn(e):
            ins = None
            for k in range(kc):
                ins = e.matmul(bank_ap, lhsT=slab[:, k, col0:col0 + 128], rhs=act[:, k, t0:t0 + nt],
                               start=(k == 0), stop=(k == kc - 1))
            return ins
        op('pe', fn, reads, writes)

    def mm_tm(bank_ap, act, t0, slab, c0, cw_, kc, reads, writes):
        def fn(e):
            ins = None
            for k in range(kc):
                ins = e.matmul(bank_ap, lhsT=act[:, k, t0:t0 + 128], rhs=slab[:, k, c0:c0 + cw_],
                               start=(k == 0), stop=(k == kc - 1))
            return ins
        op('pe', fn, reads, writes)

    PSK = lambda b: ('ps', b)

    def norm_transpose(src_tile_ap, src_key, gfm, dstT, dst_t0, dst_keys, xi, sidx, xreg='A', use_pool=False, defer_back=False):
        ss = stat[:, sidx:sidx + 1]; rs = stat[:, sidx + 1:sidx + 2]
        sk = ('stat', sidx)
        xk = K(xreg, 'xs', xi)
        xs_ap = xs[xi]
        op('act', lambda e: e.activation(out=xs_ap, in_=src_tile_ap, func=AF.Square, accum_out=ss),
           [src_key], [xk, sk])

        def rstd_fn(e):
            return e.tensor_scalar(out=rs, in0=ss, scalar1=1.0 / D, scalar2=EPS, op0=ALU.mult, op1=ALU.add)
        op('dve', rstd_fn, [sk], [('stat', sidx + 1)])
        if use_pool:
            op('pool', lambda e: e.tensor_tensor(out=rs, in0=rs, in1=nhalf[:, 0:1], op=ALU.pow), [('stat', sidx + 1)] + INIT,
               [('stat', sidx + 1)])
            op('pool', lambda e: e.tensor_scalar(out=xs_ap, in0=src_tile_ap, scalar1=rs, scalar2=1.0, op0=ALU.mult, op1=ALU.mult),
               [src_key, ('stat', sidx + 1)], [xk])
        else:
            op('act', lambda e: e.activation(out=rs, in_=rs, func=AF.Sqrt), [('stat', sidx + 1)], [('stat', sidx + 1)])
            op('dve', lambda e: e.reciprocal(out=rs, in_=rs), [('stat', sidx + 1)], [('stat', sidx + 1)])
            op('act', lambda e: e.activation(out=xs_ap, in_=src_tile_ap, func=AF.Copy, scale=rs),
               [src_key, ('stat', sidx + 1)], [xk])
        nb = (KC * 128 + 1023) // 1024
        def back():
            def tr_fn(e):
                ins = None
                for k in range(KC):
                    bank = k // 8
                    ins = e.transpose(out=pbf(bank)[:, (k % 8) * 128:(k % 8 + 1) * 128],
                                      in_=xs_ap[:, k * 128:(k + 1) * 128], identity=identb[:])
                return ins
            op('pe', tr_fn, [xk] + INIT, [PSK(b) for b in range(nb)])
            for b in range(nb):
                k0 = b * 8; k1 = min(KC, k0 + 8)
                def ev_fn(e, b=b, k0=k0, k1=k1):
                    return e.tensor_tensor(
                        out=dstT[:, k0:k1, dst_t0:dst_t0 + 128],
                        in0=pbf(b)[:, 0:(k1 - k0) * 128].rearrange("p (k t) -> p k t", k=k1 - k0),
                        in1=gfm[:, k0:k1].unsqueeze(2).broadcast_to([128, k1 - k0, 128]), op=ALU.mult)
                op('dve', ev_fn, [PSK(b)] + INIT, dst_keys)
        if defer_back:
            return back
        back()
        return None

    dma_cast(wk_s.rearrange("p a b -> p (a b)"), wk_d, [], [K('PCN', 'wk')], 'pck')
    wsrc = w_in
    slab_m = []; slab_mv = []
    for j in range(max(1, GW // 512)):
        cbw = min(512, GW)
        slab_m.append(slab_load(wsrc[:, c_m + j * cbw:c_m + (j + 1) * cbw], KC, cbw)[0:2])
    for j in range(max(1, GW // 512)):
        cbw = min(512, GW)
        slab_mv.append(slab_load(wsrc[:, c_mv + j * cbw:c_mv + (j + 1) * cbw], KC, cbw)[0:2])
    CBW = min(512, GW)
    CPS = CBW // 128

    op('dve', lambda e: e.memset(Cst.rearrange("p a b -> p (a b)"), 0.0), [], [K('B', 'Cst', hh) for hh in range(H)])
    def vinit(e):
        e.memset(vsb.rearrange("p a b c -> p (a b c)"), 1.0)
        return e.memset(Cb.rearrange("p a b -> p (a b)"), 0.0)
    op('dve', vinit, [], [K('C', 'vsb'), K('C', 'Cb')])

    R = lambda i: rows[:, i * 128:(i + 1) * 128]
    m_slot = [rsm[:, 0:1], rsm[:, 1:2]]
    xdma = dict(n=0)


    cols4 = sb("cols4", [128, 4 * 4 * H], F32)
    dsb4 = sb("dsb4", [128, 4 * 2 * H], F32)
    Mr4 = sb("Mr4", [H, 4 * 128], F32)
    tacc2 = sb("tacc2", [128, BLK], F32)
    ksb = sb("ksb", [128, MC], F32)
    nbm = sb("nbm", [H, NCHT], F32)
    nhalf = sb("nhalf", [128, 1], F32)
    dgt = sb("dgt", [H, 4 * 2 * H], F32)
    taccs = [tacc, tacc2[:]]
    fmb = dict(n=0)
    FMB = [2, 6, 7]

    DBL = XST1_OK and xs1 is not None
    def s1_tile(b, i, defer=False):
        r0 = b * BLK + i * 128
        par = (i % 2) if (DBL and b < NBP) else 0
        if par == 0:
            xkey = K('A', 'xst', 0)
            dma_sp(xst[0], xseq[r0:r0 + 128, :], [], [xkey], 'xst0')
            xs[0] = xs0_p1
            return norm_transpose(xst[0], xkey, g1s, xnTb, i * 128, [K('A', 'xnTb')], 0, 0, use_pool=True, defer_back=defer)
        else:
            xkey = K('PCN', 'xst', 1)
            dma_sp(xst1, xseq[r0:r0 + 128, :], [], [xkey], 'xst1')
            xs[1] = xs1
            return norm_transpose(xst1, xkey, g1s, xnTb, i * 128, [K('A', 'xnTb')], 1, 2, xreg='C', use_pool=True, defer_back=defer)

    xs0_p1 = xs[0]

    def p1_block(b):
        own = b >= NBP
        ob_ = b - NBP
        XB = K('A', 'xnTb')
        if b == NBP:
            dma_cast(wq_s.rearrange("p a b -> p (a b)"), wq_d, [K('PCN', 'xst', 1)], [K('PCN', 'wq'), K('PCN', 'xst', 1)], 'pcq')
            dma_sp(ngbc, ngbc_d, [], [K('PCN', 'ng'), K('PCN', 'xst', 1)], 'pcg')
        if b == 0:
            for i in range(4):
                s1_tile(b, i)
        def gate_fn(e):
            ins = None
            for gi in range(2):
                for k in range(KC):
                    ins = e.matmul(PB[3 + gi][0:H, 0:BLK], lhsT=wg[:, k * 2 * H + gi * H:k * 2 * H + (gi + 1) * H],
                                   rhs=xnTb[:, k, :], start=(k == 0), stop=(k == KC - 1))
            return ins
        op('pe', gate_fn, [XB] + INIT, [PSK(3), PSK(4)])
        def gcp(e):
            e.activation(out=gblk[:, 0:BLK], in_=PB[3][0:H, 0:BLK], func=AF.Identity, bias=igb[:])
            return e.activation(out=gblk[:, BLK:2 * BLK], in_=PB[4][0:H, 0:BLK], func=AF.Exp, scale=-1.0, bias=nfgb[:])
        op('act', gcp, [PSK(3), PSK(4)] + INIT, [('A', 'gi'), ('A', 'gf')])

        def s3(j):
            sl, skey = slab_m[j // CPS]
            bk = FMB[fmb['n'] % 3]; fmb['n'] += 1
            mm_fm(PB[bk][:, 0:BLK], sl, (j % CPS) * 128, xnTb, 0, BLK, KC, [XB, skey], [PSK(bk)])
            mb = mbuf[j % 2]; mk = K('C', 'mbuf', j % 2)
            def mev(e):
                e.copy(out=mb[:, 0:3], in_=halo[:, j * 4:j * 4 + 3])
                return e.copy(out=mb[:, 3:3 + BLK], in_=PB[bk][:, 0:BLK])
            op('act', mev, [PSK(bk), ('halo', j)] + INIT, [mk])
            op('act', lambda e: e.copy(out=halo[:, j * 4:j * 4 + 3], in_=PB[bk][:, BLK - 3:BLK]), [PSK(bk)], [('halo', j)])
            ta = taccs[j % 2]; tk = ('tacc', j % 2)
            op('act', lambda e: e.activation(out=ta, in_=mb[:, 0:BLK], func=AF.Identity, scale=cw[:, j * 4:j * 4 + 1],
                                             bias=cb[:, j:j + 1]), [mk] + INIT, [tk])
            for tap in range(1, 4):
                op('dve', lambda e, tap=tap: e.scalar_tensor_tensor(
                    out=ta, in0=mb[:, tap:tap + BLK], scalar=cw[:, j * 4 + tap:j * 4 + tap + 1], in1=ta,
                    op0=ALU.mult, op1=ALU.add), [mk, tk] + INIT, [tk])
            return lambda: op('act', lambda e: e.activation(out=cT[:, j, :], in_=ta, func=AF.Silu), [tk], [K('A', 'cT', j)])
        CTK = [K('A', 'cT', j) for j in range(MC)]

        def GB():
            gi = gblk[:, 0:BLK]; gf = gblk[:, BLK:2 * BLK]
            bcum = rows[:, 0:BLK]; gg = rows[:, BLK:2 * BLK]; Gc = rows[:, 2 * BLK:3 * BLK]
            v3 = lambda ap: ap.rearrange("p (c t) -> p c t", c=4)
            P_ = b % 2
            mm = rsm[:, 16 + 8 * P_:16 + 8 * P_ + 5]; mmo = rsm[:, 16 + 8 * (1 - P_):16 + 8 * (1 - P_) + 5]
            mprev = mm[:, 0:4]; mnew = mm[:, 1:5]
            mk4 = mask[:, b * 4:(b + 1) * 4]; nb4 = nbm[:, b * 4:(b + 1) * 4]
            bl = bcum[:, 127:BLK:128]; Gl = Gc[:, 127:BLK:128]
            am = rsm[:, 32:36]; blp = rsm[:, 36:40]; amp = rsm[:, 40:44]; d12 = rsm[:, 44:52]; ds = rsm[:, 52:60]
            KQ = lambda n: ('gq', n)
            op('act', lambda e: e.activation(out=gf, in_=gf, func=AF.Ln, bias=1.0), [('A', 'gf')], [('A', 'gf')])
            yield
            def scan1(e):
                ins = None
                for c in range(4):
                    ins = e.tensor_tensor_scan(out=bcum[:, c * 128:(c + 1) * 128], data0=onesH[:], data1=gf[:, c * 128:(c + 1) * 128],
                                               initial=0.0, op0=ALU.mult, op1=ALU.subtract)
                return ins
            op('dve', scan1, [('A', 'gf')] + INIT, [('A', 'bcum')])
            op('dve', lambda e: e.tensor_tensor(out=gg, in0=gi, in1=bcum, op=ALU.subtract), [('A', 'gi'), ('A', 'bcum')], [('A', 'gg')])
            yield
            def scan2(e):
                ins = None
                for c in range(4):
                    ins = e.tensor_tensor_scan(out=Gc[:, c * 128:(c + 1) * 128], data0=gg[:, c * 128:(c + 1) * 128],
                                               data1=gg[:, c * 128:(c + 1) * 128], initial=NEG, op0=ALU.max, op1=ALU.max)
                return ins
            op('dve', scan2, [('A', 'gg')], [('A', 'Gc')])
            def sca(e):
                e.tensor_tensor(out=am, in0=bl, in1=Gl, op=ALU.add)
                return e.tensor_tensor(out=blp, in0=bl, in1=mk4, op=ALU.mult)
            op('dve', sca, [('A', 'bcum'), ('A', 'Gc')] + INIT, [KQ('am'), KQ('blp')])
            yield
            def scb(e):
                e.tensor_tensor(out=amp, in0=am, in1=mk4, op=ALU.mult)
                return e.tensor_tensor(out=blp, in0=blp, in1=nb4, op=ALU.add)
            op('dve', scb, [KQ('am'), KQ('blp')] + INIT, [KQ('amp'), KQ('blp')])
            if b > 0:
                op('dve', lambda e: e.tensor_copy(out=mm[:, 0:1], in_=mmo[:, 4:5]), [('mm', 1 - P_)], [('mm0', P_)])
            op('dve', lambda e: e.tensor_tensor_scan(out=mnew, data0=blp, data1=amp, initial=mm[:, 0:1], op0=ALU.add, op1=ALU.max),
               [KQ('amp'), KQ('blp'), ('mm0', P_)] + INIT, [('mm', P_)])
            yield
            def scc(e):
                e.tensor_tensor(out=d12[:, 0:4], in0=blp, in1=mprev, op=ALU.add)
                return e.tensor_tensor(out=d12[:, 4:8], in0=amp, in1=mnew, op=ALU.subtract)
            op('dve', scc, [KQ('amp'), KQ('blp'), ('mm', P_), ('mm0', P_)] + INIT, [KQ('d12a')])
            op('dve', lambda e: e.tensor_tensor(out=d12[:, 0:4], in0=d12[:, 0:4], in1=mnew, op=ALU.subtract),
               [KQ('d12a'), ('mm', P_)], [KQ('d12')])
            op('act', lambda e: e.activation(out=ds, in_=d12, func=AF.Exp), [KQ('d12'), KQ('d12a')], [KQ('ds')])
            yield
            op('dve', lambda e: e.tensor_tensor(out=ds[:, 4:8], in0=ds[:, 4:8], in1=mk4, op=ALU.mult), [KQ('ds')] + INIT, [KQ('ds2')])
            dg3 = dgt[:].rearrange("p (c n) -> p c n", c=4)
            idb = identf[0:H, 0:H].unsqueeze(1).broadcast_to([H, 4, H])
            def scd(e):
                e.tensor_tensor(out=dg3[:, :, 0:H], in0=idb, in1=ds[:, 0:4].unsqueeze(2).broadcast_to([H, 4, H]), op=ALU.mult)
                return e.tensor_tensor(out=dg3[:, :, H:2 * H], in0=idb, in1=ds[:, 4:8].unsqueeze(2).broadcast_to([H, 4, H]),
                                       op=ALU.mult)
            op('dve', scd, [KQ('ds'), KQ('ds2')] + INIT, [KQ('dgt')])
            op('dve', lambda e: e.tensor_tensor(out=v3(gi), in0=v3(gg), in1=Gl.unsqueeze(2).broadcast_to([H, 4, 128]),
                                                op=ALU.subtract), [('A', 'gg'), ('A', 'Gc'), ('A', 'gi')], [('A', 'gi')])
            op('act', lambda e: e.activation(out=gi, in_=gi, func=AF.Exp), [('A', 'gi')], [('A', 'wa')])
            yield
            if own:
                mpb = mprev.unsqueeze(2).broadcast_to([H, 4, 128])
                op('dve', lambda e: e.tensor_tensor(out=v3(Mr4[:]), in0=v3(Gc), in1=mpb, op=ALU.max),
                   [('A', 'Gc'), ('mm', P_), ('mm0', P_)], [('Mr',)])
                op('dve', lambda e: e.tensor_tensor(out=v3(gf), in0=mpb, in1=v3(Mr4[:]), op=ALU.subtract),
                   [('Mr',), ('mm', P_), ('mm0', P_), ('A', 'gf'), ('A', 'bcum')], [('A', 'gf')])
                op('dve', lambda e: e.tensor_tensor(out=bcum, in0=bcum, in1=Mr4[:], op=ALU.add),
                   [('Mr',), ('A', 'bcum'), KQ('am'), KQ('blp')], [('A', 'bcum')])
                def ex2(e):
                    e.activation(out=gf, in_=gf, func=AF.Exp)
                    return e.activation(out=bcum, in_=bcum, func=AF.Exp, scale=-1.0)
                op('act', ex2, [('A', 'gf'), ('A', 'bcum')], [('A', 'winter'), ('A', 'emt')])
            yield 'pe'
            def bc_fn(e):
                ins = e.matmul(PB[5][:, 0:8 * H], lhsT=onesH[:], rhs=dgt[:], start=True, stop=True)
                for c in range(4):
                    cs_ = slice(c * 128, (c + 1) * 128); c0 = 64 + c * 4 * H
                    ins = e.transpose(out=PB[5][:, c0:c0 + H], in_=gi[:, cs_], identity=identf[0:H, 0:H])
                    if own:
                        e.transpose(out=PB[5][:, c0 + H:c0 + 2 * H], in_=gg[:, cs_], identity=identf[0:H, 0:H])
                        e.transpose(out=PB[5][:, c0 + 2 * H:c0 + 3 * H], in_=gf[:, cs_], identity=identf[0:H, 0:H])
                        ins = e.transpose(out=PB[5][:, c0 + 3 * H:c0 + 4 * H], in_=bcum[:, cs_], identity=identf[0:H, 0:H])
                return ins
            op('pe', bc_fn, [KQ('dgt'), ('A', 'wa'), ('A', 'gg'), ('A', 'winter'), ('A', 'emt')] + INIT, [PSK(5)])
            ncol = 4 * H if own else H
            def bc_ev(e):
                e.tensor_copy(out=dsb4[:, 0:8 * H], in_=PB[5][:, 0:8 * H])
                return e.tensor_copy(out=cols4[:].rearrange("p (c n) -> p c n", c=4)[:, :, 0:ncol],
                                     in_=PB[5][:, 64:64 + 16 * H].rearrange("p (c n) -> p c n", c=4)[:, :, 0:ncol])
            op('dve', bc_ev, [PSK(5)], [('dsb', i_) for i_ in range(4)] + [('cols', i_) for i_ in range(4)])

        gbg = GB()
        gb_state = dict(at_pe=False, done=False)
        def gb_step(allow_pe=False):
            if gb_state['done'] or (gb_state['at_pe'] and not allow_pe):
                return False
            try:
                r = next(gbg)
                if r == 'pe':
                    gb_state['at_pe'] = True
                return True
            except StopIteration:
                gb_state['done'] = True
                return False
        pend = None
        for j in range(MC):
            nxt = s3(j)
            if pend is not None:
                pend()
            pend = nxt
        pend()

        if own:
            for (wsx, dst, dkey, scale) in ((wq_s, qT, 'qT', 1.0), (wk_s, kT, 'kT', RS)):
                for hh in range(H):
                    for ec in range(DC):
                        bk = FMB[fmb['n'] % 3]; fmb['n'] += 1
                        def fn(e, wsx=wsx, hh=hh, ec=ec, bk=bk):
                            ins = None
                            for dc in range(DC):
                                ins = e.matmul(PB[bk][:, 0:BLK], lhsT=wsx[:, hh * DC + dc, ec * 128:(ec + 1) * 128],
                                               rhs=cT[:, hh * DC + dc, :], start=(dc == 0), stop=(dc == DC - 1))
                            return ins
                        op('pe', fn, CTK + [K('PCN', 'wq'), K('PCN', 'wk')], [PSK(bk)])
                        reg = 'A' if dkey == 'qT' else 'C'
                        op('act', lambda e, dst=dst, hh=hh, ec=ec, scale=scale, bk=bk: e.activation(
                            out=dst[:, hh * DC + ec, :], in_=PB[bk][:, 0:BLK], func=AF.Copy, scale=scale),
                           [PSK(bk)], [K(reg, dkey, hh * DC + ec)])

        def V(i):
            for j in range(len(slab_mv)):
                sl, skey = slab_mv[j]
                bk = [6, 7][j % 2]
                mm_tm(PB[bk][:, 0:CBW], xnTb, i * 128, sl, 0, CBW, KC, [XB, skey], [PSK(bk)])
                nh = CBW // Dh
                op('act', lambda e, j=j, nh=nh, bk=bk: e.copy(out=vsb[:, i, j * nh:(j + 1) * nh, 0:Dh],
                                                        in_=PB[bk][:, 0:CBW].rearrange("p (h d) -> p h d", h=nh)),
                   [PSK(bk)], [K('C', 'vsb', i, j)])
        for i in range(4):
            V(i)
            gb_step(); gb_step()
        while gb_step(allow_pe=True):
            pass

        def KOU(i):
            ti = ob_ * 4 + i
            csl = slice(i * 128, (i + 1) * 128)
            colsI = cols4[:, i * 4 * H:(i + 1) * 4 * H]
            dsbI = dsb4[:, i * 2 * H:(i + 1) * 2 * H]
            Mr = Mr4[:, i * 128:(i + 1) * 128]
            CK = ('cols', i); DK = ('dsb', i)
            for hp in range(0, H, 2):
                bk = [2, 6][(hp // 2) % 2]
                def kfn(e, hp=hp, bk=bk):
                    ins = None
                    for hh in range(hp, hp + 2):
                        for dc in range(DC):
                            ins = e.matmul(PB[bk][:, (hh - hp) * Dh:(hh - hp + 1) * Dh],
                                           lhsT=cT[:, hh * DC + dc, csl], rhs=wk_s[:, hh * DC + dc, :],
                                           start=(dc == 0), stop=(dc == DC - 1))
                    return ins
                op('pe', kfn, CTK + [K('PCN', 'wk')], [PSK(bk)])
                def kev(e, hp=hp, bk=bk):
                    ins = None
                    for hh in range(hp, hp + 2):
                        ins = e.tensor_scalar(out=kw[:, i, hh, :], in0=PB[bk][:, (hh - hp) * Dh:(hh - hp + 1) * Dh],
                                              scalar1=colsI[:, hh:hh + 1], scalar2=RS, op0=ALU.mult, op1=ALU.mult)
                    return ins
                op('dve', kev, [PSK(bk), CK], [K('A', 'kw', i, hp)])
            VK = [K('C', 'vsb', i, j) for j in range(len(slab_mv))] + [K('C', 'vsb')]
            KWK = [K('A', 'kw', i, hp) for hp in range(0, H, 2)]
            if own:
                def cbf(e):
                    e.copy(out=Cb[:, :, 0:Dh], in_=Cst)
                    return e.copy(out=Cb[:, :, Dh:Dh + 1], in_=nst[:].unsqueeze(2))
                op('act', cbf, [K('B', 'Cst', hh) for hh in range(H)] + [('nst', hh) for hh in range(H)] + INIT, [K('C', 'Cb')])
                tcs = csl
                def sfn(e):
                    ins = None
                    for hh in range(H):
                        for dc in range(DC):
                            ins = e.matmul(PB[3][:, hh * 128:(hh + 1) * 128], lhsT=kT[:, hh * DC + dc, tcs],
                                           rhs=qT[:, hh * DC + dc, tcs], start=(dc == 0), stop=(dc == DC - 1))
                    for hh in range(H):
                        ins = e.matmul(PB[4][:, hh * 128:(hh + 1) * 128], lhsT=sel[:, hh * 128:(hh + 1) * 128],
                                       rhs=Mr, start=True, stop=True)
                    return ins
                op('pe', sfn, [K('A', 'qT', c) for c in range(MC)] + [K('C', 'kT', c) for c in range(MC)]
                   + [('Mr',), ('A', 'sel')] + INIT, [PSK(3), PSK(4)])
                def zfn(e):
                    ins = None
                    for hh in range(H):
                        ins = e.tensor_scalar(out=zt[:, hh, :], in0=PB[4][:, hh * 128:(hh + 1) * 128],
                                              scalar1=colsI[:, H + hh:H + hh + 1], scalar2=None, op0=ALU.max)
                    return ins
                op('dve', zfn, [PSK(4), CK], [K('B', 'zt')])
                def efn(e):
                    ins = None
                    for hh in range(H):
                        ins = e.activation(out=Ebuf[:, hh, :], in_=zt[:, hh, :], func=AF.Exp, scale=-1.0,
                                           bias=colsI[:, H + hh:H + hh + 1])
                    return ins
                op('act', efn, [K('B', 'zt'), CK], [K('B', 'E')])
                op('dve', lambda e: e.tensor_tensor(out=Ebuf, in0=Ebuf,
                                                    in1=tri[:].unsqueeze(1).broadcast_to([128, H, 128]), op=ALU.mult),
                   [K('B', 'E')] + INIT, [K('B', 'E')])
                op('dve', lambda e: e.tensor_tensor(out=PT.rearrange("p a b -> p (a b)"),
                                                    in0=Ebuf.rearrange("p a b -> p (a b)"), in1=PB[3][:, 0:H * 128],
                                                    op=ALU.mult),
                   [K('B', 'E'), PSK(3)], [K('B', 'PT')])
                dent = stat[:, 24:24 + H]; dt2 = stat[:, 32:32 + H]; rden = stat[:, 40:40 + H]
                mvt = bnst[:, 0:0]
                IB = [6, 2]; EB = [7, 1]
                for hh in range(H):
                    def nfn(e, hh=hh):
                        e.matmul(PB[IB[hh % 2]][:, 0:Dh + 1], lhsT=PT[:, hh, :], rhs=vsb[:, i, hh, 0:Dh + 1], start=True, stop=True)
                        ins = None
                        for dc in range(DC):
                            ins = e.matmul(PB[EB[hh % 2]][:, 0:Dh + 1], lhsT=qT[:, hh * DC + dc, tcs],
                                           rhs=Cb[:, hh * DC + dc, 0:Dh + 1], start=(dc == 0), stop=(dc == DC - 1))
                        return ins
                    op('pe', nfn, [K('B', 'PT'), K('C', 'Cb')] + VK + [K('A', 'qT', c) for c in range(MC)],
                       [PSK(IB[hh % 2]), PSK(EB[hh % 2])])
                    hsl = slice(hh * Dh, (hh + 1) * Dh)
                    def icp(e, hh=hh, hsl=hsl):
                        e.copy(out=hbuf[:, hsl], in_=PB[IB[hh % 2]][:, 0:Dh])
                        return e.copy(out=dt2[:, hh:hh + 1], in_=PB[IB[hh % 2]][:, Dh:Dh + 1])
                    op('act', icp, [PSK(IB[hh % 2])], [K('C', 'hbuf', hh), ('dt2', hh), K('C', 'xs', 1)])
                    def cmb(e, hh=hh, hsl=hsl):
                        e.scalar_tensor_tensor(out=hbuf[:, hsl], in0=PB[EB[hh % 2]][:, 0:Dh],
                                               scalar=colsI[:, 2 * H + hh:2 * H + hh + 1], in1=hbuf[:, hsl],
                                               op0=ALU.mult, op1=ALU.add)
                        return e.scalar_tensor_tensor(out=dent[:, hh:hh + 1], in0=PB[EB[hh % 2]][:, Dh:Dh + 1],
                                                      scalar=colsI[:, 2 * H + hh:2 * H + hh + 1], in1=dt2[:, hh:hh + 1],
                                                      op0=ALU.mult, op1=ALU.add)
                    op('dve', cmb, [PSK(EB[hh % 2]), K('C', 'hbuf', hh), ('dt2', hh), CK], [K('C', 'hbuf', hh), ('dent', hh)])
                DENK = [('dent', hh) for hh in range(H)]
                op('act', lambda e: e.activation(out=rden, in_=dent, func=AF.Abs), DENK, [('rden',)])
                op('dve', lambda e: e.tensor_tensor(out=rden, in0=rden, in1=colsI[:, 3 * H:4 * H], op=ALU.max),
                   [('rden',), CK], [('rden',)])
                op('dve', lambda e: e.reciprocal(out=rden, in_=rden), [('rden',)], [('rden',)])
                for hh in range(H):
                    hsl = slice(hh * Dh, (hh + 1) * Dh)
                    op('act', lambda e, hh=hh, hsl=hsl: e.activation(out=hbuf[:, hsl], in_=hbuf[:, hsl], func=AF.Copy,
                                                                   scale=rden[:, hh:hh + 1]),
                       [K('C', 'hbuf', hh), ('rden',)], [K('C', 'hbuf', hh)])
                    op('dve', lambda e, hh=hh, hsl=hsl: e.bn_stats(out=bnst[:, hh * 6:(hh + 1) * 6], in_=hbuf[:, hsl]),
                       [K('C', 'hbuf', hh)], [('bnst', hh)])
                mv = stat[:, 48:48 + 2 * H]; rsd = stat[:, 56:56 + H]
                for hh in range(H):
                    op('dve', lambda e, hh=hh: e.bn_aggr(out=mv[:, 2 * hh:2 * hh + 2], in_=bnst[:, hh * 6:(hh + 1) * 6]),
                       [('bnst', hh)], [('mv', hh)])
                MVK = [('mv', hh) for hh in range(H)]
                op('dve', lambda e: e.tensor_scalar(out=rsd, in0=mv[:, 1:2 * H:2], scalar1=EPS, scalar2=None, op0=ALU.add),
                   MVK, [('rsd',)])
                op('act', lambda e: e.activation(out=rsd, in_=rsd, func=AF.Sqrt), [('rsd',)], [('rsd',)])
                op('dve', lambda e: e.reciprocal(out=rsd, in_=rsd), [('rsd',)], [('rsd',)])
                for hh in range(H):
                    hsl = slice(hh * Dh, (hh + 1) * Dh)
                    op('dve', lambda e, hh=hh, hsl=hsl: e.tensor_scalar(out=hbuf[:, hsl], in0=hbuf[:, hsl],
                                                                       scalar1=mv[:, 2 * hh:2 * hh + 1], scalar2=rsd[:, hh:hh + 1],
                                                                       op0=ALU.subtract, op1=ALU.mult),
                       [K('C', 'hbuf', hh), ('mv', hh), ('rsd',)], [K('C', 'hbuf', hh)])
                op('dve', lambda e: e.tensor_tensor(out=hn_store[:, ti, :], in0=hbuf, in1=ngbc, op=ALU.mult),
                   [K('C', 'hbuf', hh) for hh in range(H)] + [K('PCN', 'ng')], [K('B', 'hn', ti)])
            def ksfn(e):
                ins = None
                for hh in range(H):
                    for dc in range(DC):
                        ins = e.matmul(PB[5][:, 256 + hh * DC + dc:256 + hh * DC + dc + 1],
                                       lhsT=kw[:, i, hh, dc * 128:(dc + 1) * 128], rhs=onesb[:], start=True, stop=True)
                return ins
            op('pe', ksfn, KWK + INIT, [PSK(5)])
            op('act', lambda e: e.copy(out=ksb[:, 0:MC], in_=PB[5][:, 256:256 + MC]), [PSK(5)], [('ksb',)])
            for hh in range(H):
                bk = [7, 2][hh % 2]
                def kvfn(e, hh=hh, bk=bk):
                    ins = None
                    for dc in range(DC):
                        ins = e.matmul(PB[bk][:, dc * Dh:(dc + 1) * Dh],
                                       lhsT=kw[:, i, hh, dc * 128:(dc + 1) * 128], rhs=vsb[:, i, hh, 0:Dh], start=True, stop=True)
                    return ins
                op('pe', kvfn, KWK + VK + INIT, [PSK(bk)])
                csl_ = slice(hh * DC, (hh + 1) * DC)
                SK = K('B', 'Cst', hh); NK = ('nst', hh)
                def upd(e, hh=hh, csl_=csl_):
                    cs = Cst[:, csl_, :].rearrange("p a b -> p (a b)")
                    e.tensor_scalar(out=cs, in0=cs, scalar1=dsbI[:, hh:hh + 1], scalar2=1.0, op0=ALU.mult, op1=ALU.mult)
                    return e.tensor_scalar(out=nst[:, csl_], in0=nst[:, csl_], scalar1=dsbI[:, hh:hh + 1], scalar2=1.0,
                                           op0=ALU.mult, op1=ALU.mult)
                op('pool', upd, [SK, NK, DK] + INIT, [SK, NK])
                def upd2(e, hh=hh, csl_=csl_, bk=bk):
                    cs = Cst[:, csl_, :].rearrange("p a b -> p (a b)")
                    e.scalar_tensor_tensor(out=cs, in0=PB[bk][:, 0:DC * Dh], scalar=dsbI[:, H + hh:H + hh + 1], in1=cs,
                                           op0=ALU.mult, op1=ALU.add)
                    return e.scalar_tensor_tensor(out=nst[:, csl_], in0=ksb[:, csl_],
                                                  scalar=dsbI[:, H + hh:H + hh + 1], in1=nst[:, csl_], op0=ALU.mult, op1=ALU.add)
                op('dve', upd2, [PSK(bk), ('ksb',), SK, NK, DK], [SK, NK])

        for i in range(4):
            bk_ = s1_tile(b + 1, i, defer=True) if b + 1 < NB else None
            KOU(i)
            if bk_ is not None:
                bk_()

    for b in range(NB):
        p1_block(b)


    region_barrier('A')
    region_barrier('B')
    region_barrier('C')
    region_barrier('PCN')
    dma_sp(lng_s, lngbc, [], [K('PCN', 'ln')], 'pcn2')
    dma_sp(lnb_s, lnbbc, [], [K('PCN', 'ln')], 'pcn2')
    dma_sp(bs_s, bsbc, [], [K('PCN', 'ln')], 'pcn2')
    LNK = [K('PCN', 'ln')]

    stream = []
    def add_stream(src, kc, cbw, tag):
        stream.append(dict(src=src, kc=kc, cbw=cbw, tag=tag, loaded=None))
    nsl = max(1, GW // 512)
    for j in range(nsl): add_stream(w_in[:, c_o + j * CBW:c_o + (j + 1) * CBW], KC, CBW, ('o', j))
    for j in range(nsl): add_stream(w_in[:, c_v + j * CBW:c_v + (j + 1) * CBW], KC, CBW, ('v', j))
    for j in range(nsl): add_stream(w_in[:, c_u + j * CBW:c_u + (j + 1) * CBW], KC, CBW, ('u', j))
    DB = min(512, D)
    ABW = min(D, SL // MC)
    for j in range(D // DB):
        add_stream(w_in[:, c_ga + j * DB:c_ga + (j + 1) * DB], KC, DB, ('ga', j))
        if (j * DB) % ABW == 0:
            add_stream(w_a[:, j * DB:j * DB + ABW], MC, ABW, ('wa', (j * DB) // ABW))
    for j in range(D // DB):
        add_stream(w_in[:, c_gb + j * DB:c_gb + (j + 1) * DB], KC, DB, ('gb', j))
        if (j * DB) % ABW == 0:
            add_stream(w_b[:, j * DB:j * DB + ABW], MC, ABW, ('wb', (j * DB) // ABW))
    for hf in range(2):
        for j in range(D // DB): add_stream(w_out[:, j * DB:(j + 1) * DB], KC, DB, ('wo', hf, j))
        for gq in range(NG):
            for j in range(FG // DB): add_stream(w_ff1[:, gq * FG + j * DB:gq * FG + (j + 1) * DB], KC, DB, ('f1', hf, gq, j))
            for j in range(D // DB): add_stream(w_ff2[gq * FG:(gq + 1) * FG, j * DB:(j + 1) * DB], KF, DB, ('f2', hf, gq, j))
    spos = dict(issued=0)
    sidx = {s['tag']: n for n, s in enumerate(stream)}

    def get_slab(tag, look=2):
        n = sidx[tag]
        upto = min(len(stream), n + 1 + look)
        while spos['issued'] < upto:
            s = stream[spos['issued']]
            s['loaded'] = slab_load(s['src'], s['kc'], s['cbw'])
            spos['issued'] += 1
        v_, k_, g_ = stream[n]['loaded']
        assert ring_state['n'] <= g_ + 4, ("slab overwritten before use", tag)
        return v_, k_

    for t in range(NT):
        xi = xdma['n'] % 2; xdma['n'] += 1
        r0 = NPREV + t * 128
        xkey = K('A', 'xst2', xi)
        stg = C_f[:, 6144:6144 + D]
        dma_sp(stg, xseq[r0:r0 + 128, :], [], [K('C', 'stg', 0)], 'xst0')
        xs[0] = B[:, NT * GW:NT * GW + D]
        xs[1] = B[:, NT * GW + D:NT * GW + 2 * D]
        norm_transpose(stg, K('C', 'stg', 0), g1s, xnT, t * 128, [K('A', 'xnT', t)], xi, 2 * xi, xreg='B')
    XNK = [K('A', 'xnT', t) for t in range(NT)]
    XSK = []
    XSB = [K('B', 'xs', 0), K('B', 'xs', 1)]

    def gelu_from_psum(bank_ap, n, par, out_ap, out_keys, extra_reads=()):
        x_ = gx[par][:, 0:n]; t_ = gt[par][:, 0:n]; z_ = gz[par][:, 0:n]
        kx = K('C', 'gx', par); kt = K('C', 'gt', par); kz = K('C', 'gz', par)
        op('act', lambda e: e.copy(out=x_, in_=bank_ap), list(extra_reads), [kx])
        op('dve', lambda e: e.scalar_tensor_tensor(out=t_, in0=x_, scalar=0.044715, in1=x_, op0=ALU.mult, op1=ALU.mult),
           [kx], [kt])
        op('dve', lambda e: e.scalar_tensor_tensor(out=z_, in0=t_, scalar=1.0, in1=x_, op0=ALU.add, op1=ALU.mult),
           [kt, kx], [kz])
        op('act', lambda e: e.activation(out=z_, in_=z_, func=AF.Sigmoid, scale=1.5957691216057308), [kz], [kz])
        op('dve', lambda e: e.tensor_tensor(out=out_ap, in0=z_, in1=x_, op=ALU.mult), [kz, kx], out_keys)

    pbank = dict(n=0)
    def nextbank():
        b = pbank['n'] % 8
        pbank['n'] += 1
        return b

    for t in range(NT):
        for j in range(nsl):
            sl, skey = get_slab(('o', j))
            bk = nextbank()
            mm_tm(PB[bk][:, 0:CBW], xnT, t * 128, sl, 0, CBW, KC, [K('A', 'xnT', t), skey], [PSK(bk)])
            op('act', lambda e, bk=bk: e.activation(out=gx[0][:, 0:CBW], in_=PB[bk][:, 0:CBW], func=AF.Sigmoid),
               [PSK(bk)], [K('C', 'gx', 0)])
            op('dve', lambda e, j=j, t=t: e.tensor_tensor(out=ybtm[:, 0:CBW], in0=gx[0][:, 0:CBW],
                                                          in1=hn_store[:, t, j * CBW:(j + 1) * CBW], op=ALU.mult),
               [K('C', 'gx', 0), K('B', 'hn', t)], [K('C', 'ybtm')])
            bk2 = nextbank()
            def trf(e, bk2=bk2):
                ins = None
                for c in range(CPS):
                    ins = e.transpose(out=pbf(bk2)[:, c * 128:(c + 1) * 128], in_=ybtm[:, c * 128:(c + 1) * 128],
                                      identity=identb[:])
                return ins
            op('pe', trf, [K('C', 'ybtm')] + INIT, [PSK(bk2)])
            op('act', lambda e, bk2=bk2, j=j, t=t: e.copy(
                out=ybT[:, j * CPS:(j + 1) * CPS, t * 128:(t + 1) * 128],
                in_=pbf(bk2)[:, 0:CPS * 128].rearrange("p (c t) -> p c t", c=CPS)),
               [PSK(bk2)] + XSK, [K('A', 'ybT', t)])
    YBK = [K('A', 'ybT', t) for t in range(NT)]
    for t in range(NT):
        for j in range(nsl):
            sl, skey = get_slab(('v', j))
            bk = nextbank()
            mm_tm(PB[bk][:, 0:CBW], xnT, t * 128, sl, 0, CBW, KC, [K('A', 'xnT', t), skey], [PSK(bk)])
            gelu_from_psum(PB[bk][:, 0:CBW], CBW, j % 2, gv[:, j * CBW:(j + 1) * CBW], [K('C', 'gv', j)], [PSK(bk)])
        GVK = [K('C', 'gv', j) for j in range(nsl)]
        def bnf(e):
            ins = None
            for j in range(nsl):
                ins = e.bn_stats(out=bnst[:, j * 6:(j + 1) * 6], in_=gv[:, j * CBW:(j + 1) * CBW])
            return ins
        op('dve', bnf, GVK, [('bnst', 0)])
        op('dve', lambda e: e.bn_aggr(out=bnst[:, 12:14], in_=bnst[:, 0:6 * nsl]), [('bnst', 0)], [('bnst', 1)])
        def rs3(e):
            return e.tensor_scalar(out=bnst[:, 14:15], in0=bnst[:, 13:14], scalar1=EPS, scalar2=None, op0=ALU.add)
        op('dve', rs3, [('bnst', 1)], [('bnst', 2)])
        op('act', lambda e: e.activation(out=bnst[:, 14:15], in_=bnst[:, 14:15], func=AF.Sqrt), [('bnst', 2)], [('bnst', 2)])
        op('dve', lambda e: e.reciprocal(out=bnst[:, 14:15], in_=bnst[:, 14:15]), [('bnst', 2)], [('bnst', 2)])
        op('dve', lambda e: e.scalar_tensor_tensor(out=tm1, in0=gv, scalar=bnst[:, 12:13], in1=lng_s, op0=ALU.subtract,
                                                   op1=ALU.mult), GVK + [('bnst', 1)] + LNK, [K('C', 'tm1')])
        op('dve', lambda e, t=t: e.scalar_tensor_tensor(out=v_ln[:, t, :], in0=tm1, scalar=bnst[:, 14:15], in1=lnb_s,
                                                        op0=ALU.mult, op1=ALU.add),
           [K('C', 'tm1'), ('bnst', 2)] + LNK, [K('B', 'vln', t)] + XSB)
    for j in range(nsl):
        sl, skey = get_slab(('u', j))
        for c in range(CPS):
            for n in range(NBO):
                bk = nextbank()
                mm_fm(PB[bk][:, 0:BLK], sl, c * 128, xnT, n * BLK, BLK, KC, XNK + [skey], [PSK(bk)])
                gelu_from_psum(PB[bk][:, 0:BLK], BLK, (c + n) % 2, yaT[:, j * CPS + c, n * BLK:(n + 1) * BLK],
                               [K('A', 'yaT', j * CPS + c, n)], [PSK(bk)])
    for t in range(NT):
        nbk = (G * 128 + 511) // 512
        bks = [nextbank() for _ in range(nbk)]
        def spf(e, t=t, bks=bks):
            ins = None
            for g in range(G):
                ins = e.matmul(PB[bks[g // 4]][:, (g % 4) * 128:(g % 4 + 1) * 128], lhsT=v_ln[:, t, g * 128:(g + 1) * 128],
                               rhs=wsTm[:, g * 128:(g + 1) * 128], start=True, stop=True)
            return ins
        op('pe', spf, [K('B', 'vln', t)] + INIT, [PSK(b_) for b_ in bks])
        for q, bk in enumerate(bks):
            g0 = q * 4; g1 = min(G, g0 + 4); w_ = (g1 - g0) * 128
            op('dve', lambda e, bk=bk, g0=g0, w_=w_: e.tensor_tensor(out=tm1[:, 0:w_], in0=PB[bk][:, 0:w_],
                                                                    in1=bs_s[:, g0 * 128:g0 * 128 + w_], op=ALU.add),
               [PSK(bk)] + LNK, [K('C', 'tm1')])
            rk = [K('A', 'yaT', g, (t * 128) // BLK) for g in range(g0, g1)]
            op('dve', lambda e, g0=g0, g1=g1, t=t: e.tensor_tensor(
                out=yaT[:, g0:g1, t * 128:(t + 1) * 128], in0=tm1[:, 0:(g1 - g0) * 128].rearrange("p (g t) -> p g t", g=g1 - g0),
                in1=yaT[:, g0:g1, t * 128:(t + 1) * 128], op=ALU.mult),
               [K('C', 'tm1')] + rk, rk)
    YAK = [K('A', 'yaT', g, n) for g in range(G) for n in range(NBO)]

    region_barrier('B')
    for (gtag, wtag, yT, ykeys, gi, first) in (('ga', 'wa', yaT, YAK, 0, True), ('gb', 'wb', ybT, YBK, 1, False)):
        for j in range(KC):
            slg, kg = get_slab((gtag, (j * 128) // DB))
            slw, kw_ = get_slab((wtag, (j * 128) // ABW))
            for n in range(NBO):
                b1 = nextbank(); b2 = nextbank()
                mm_fm(PB[b1][:, 0:BLK], slg, (j * 128) % DB, xnT, n * BLK, BLK, KC, XNK + [kg], [PSK(b1)])
                mm_fm(PB[b2][:, 0:BLK], slw, (j * 128) % ABW, yT, n * BLK, BLK, MC, ykeys + [kw_], [PSK(b2)])
                par = (j + n) % 2
                op('act', lambda e, b1=b1, j=j, par=par, gi=gi: e.activation(
                    out=gx[par], in_=PB[b1][:, 0:BLK], func=AF.Sigmoid, bias=bgate[:, gi * KC + j:gi * KC + j + 1]),
                   [PSK(b1)] + INIT, [K('C', 'gx', par)])
                mk = K('B', 'mixedT', j, n)
                if first:
                    op('dve', lambda e, b2=b2, j=j, n=n, par=par: e.tensor_tensor(
                        out=mixedT[:, j, n * BLK:(n + 1) * BLK], in0=gx[par], in1=PB[b2][:, 0:BLK], op=ALU.mult),
                       [K('C', 'gx', par), PSK(b2)], [mk])
                else:
                    op('dve', lambda e, b2=b2, par=par: e.tensor_tensor(out=gt[par], in0=gx[par], in1=PB[b2][:, 0:BLK],
                                                                        op=ALU.mult),
                       [K('C', 'gx', par), PSK(b2)], [K('C', 'gt', par)])
                    op('dve', lambda e, j=j, n=n, par=par: e.tensor_tensor(
                        out=mixedT[:, j, n * BLK:(n + 1) * BLK], in0=mixedT[:, j, n * BLK:(n + 1) * BLK], in1=gt[par],
                        op=ALU.add), [K('C', 'gt', par), mk], [mk])
    MXK = [K('B', 'mixedT', j, n) for j in range(KC) for n in range(NBO)]

    for hf in range(2):
        region_barrier('A')
        if hf == 1:
            region_barrier('C')
        tok0 = NPREV + hf * HT
        for t in range(NHT):
            dma_sp(x1[:, t, :], xseq[tok0 + t * 128:tok0 + (t + 1) * 128, :], [], [K('A', 'x1', t)], 'x1_%d' % t)
        for j in range(D // DB):
            sl, skey = get_slab(('wo', hf, j))
            for t in range(NHT):
                bk = nextbank()
                mm_tm(PB[bk][:, 0:DB], mixedT, hf * HT + t * 128, sl, 0, DB, KC, MXK + [skey], [PSK(bk)])
                op('dve', lambda e, bk=bk, t=t, j=j: e.tensor_tensor(out=x1[:, t, j * DB:(j + 1) * DB],
                                                                      in0=x1[:, t, j * DB:(j + 1) * DB], in1=PB[bk][:, 0:DB],
                                                                      op=ALU.add),
                   [PSK(bk), K('A', 'x1', t)], [K('A', 'x1', t)])
        xs[0] = C[:, 8192:8192 + D]; xs[1] = C[:, 8192 + D:8192 + 2 * D]
        for t in range(NHT):
            norm_transpose(x1[:, t, :], K('A', 'x1', t), g2s, hnT, t * 128, [K('A', 'hnT', t)], t % 2, 2 * (t % 2), xreg='C')
        HNK = [K('A', 'hnT', t) for t in range(NHT)]
        for gq in range(NG):
            for j in range(FG // DB):
                sl, skey = get_slab(('f1', hf, gq, j))
                for c in range(DB // 128):
                    for n in range(HT // FB):
                        bk = nextbank()
                        mm_fm(PB[bk][:, 0:FB], sl, c * 128, hnT, n * FB, FB, KC, HNK + [skey], [PSK(bk)])
                        par = (c + n) % 2
                        op('act', lambda e, bk=bk, par=par: e.activation(out=gx[par][:, 0:FB], in_=PB[bk][:, 0:FB], func=AF.Relu),
                           [PSK(bk)], [K('C', 'gx', par)])
                        kc_ = j * (DB // 128) + c
                        op('dve', lambda e, par=par, kc_=kc_, n=n: e.tensor_tensor(
                            out=h1T[:, kc_, n * FB:(n + 1) * FB], in0=gx[par][:, 0:FB], in1=gx[par][:, 0:FB], op=ALU.mult),
                           [K('C', 'gx', par)], [K('A', 'h1T', kc_, n)])
            H1K = [K('A', 'h1T', k_, n) for k_ in range(KF) for n in range(HT // FB)]
            for j in range(D // DB):
                sl, skey = get_slab(('f2', hf, gq, j))
                for t in range(NHT):
                    bk = nextbank()
                    mm_tm(PB[bk][:, 0:DB], h1T, t * 128, sl, 0, DB, KF, H1K + [skey], [PSK(bk)])
                    op('dve', lambda e, bk=bk, t=t, j=j: e.tensor_tensor(out=x1[:, t, j * DB:(j + 1) * DB],
                                                                          in0=x1[:, t, j * DB:(j + 1) * DB],
                                                                          in1=PB[bk][:, 0:DB], op=ALU.add),
                       [PSK(bk), K('A', 'x1', t)], [K('A', 'x1', t)])
        region_barrier('C')
        dma_sp(gfb, gfbc, [], [K('C', 'gfb')], 'gfb')
        for t in range(NHT):
            ss = stat[:, 16:17]; rs = stat[:, 17:18]
            op('act', lambda e, t=t: e.activation(out=junkF, in_=x1[:, t, :], func=AF.Square, accum_out=ss),
               [K('A', 'x1', t)], [K('C', 'junkF'), ('stat', 16)])
            def rsf(e):
                return e.tensor_scalar(out=rs, in0=ss, scalar1=1.0 / D, scalar2=EPS, op0=ALU.mult, op1=ALU.add)
            op('dve', rsf, [('stat', 16)], [('stat', 17)])
            op('act', lambda e: e.activation(out=rs, in_=rs, func=AF.Sqrt), [('stat', 17)], [('stat', 17)])
            op('dve', lambda e: e.reciprocal(out=rs, in_=rs), [('stat', 17)], [('stat', 17)])
            oi = t % 2
            op('dve', lambda e, t=t, oi=oi: e.scalar_tensor_tensor(out=ost[oi], in0=x1[:, t, :], scalar=rs, in1=gfb,
                                                                   op0=ALU.mult, op1=ALU.mult),
               [K('A', 'x1', t), ('stat', 17), K('C', 'gfb')], [K('C', 'ost', oi)])
            r0 = hf * HT + t * 128
            dma_sp(out_d[r0:r0 + 128, :], ost[oi], [K('C', 'ost', oi)], [('outd', hf, t)], 'ost%d' % oi)
    S.op('sp', None, reads=[('outd', hf, t) for hf in range(2) for t in range(NHT)], writes=[])

    dnames = S.finalize()
    sems = {}
    for e_ in ('pe', 'act', 'dve', 'pool', 'sp'):
        sems[('eng', e_)] = es.enter_context(nc.semaphore("s_" + e_))
    for dn_ in dnames:
        sems[('dma', dn_)] = es.enter_context(nc.semaphore("d_" + dn_))
    with nc.Block() as block:
        @block.tensor
        def _(e):
            S.emit_engine('pe', e, sems)

        @block.scalar
        def _(e):
            S.emit_engine('act', e, sems)

        @block.vector
        def _(e):
            S.emit_engine('dve', e, sems)

        @block.gpsimd
        def _(e):
            S.emit_engine('pool', e, sems)

        @block.sync
        def _(e):
            S.emit_engine('sp', e, sems)
    es.close()
    return nc


def make_inputs(cfg, p, core):
    D = cfg['D']; T = cfg['T']; NPREV = cfg['NPREV']; H = cfg['H']
    GW = D // 2; G = GW // 128; Dh = GW // H; DC = Dh // 128; KC = D // 128; MC = GW // 128
    NCHT = (NPREV + T) // 128
    x = p['x']
    Bn, Sn, _ = x.shape
    cps = Sn // T
    b = core // cps; pos = core % cps
    xs_ = x[b]
    nreal = pos * T
    xseq = np.zeros((NPREV + T, D), np.float32)
    if nreal:
        xseq[NPREV - nreal:NPREV] = xs_[0:nreal]
    xseq[NPREV:] = xs_[nreal:nreal + T]
    mask = np.ones((H, NCHT), np.float32)
    mask[:, 0:(NPREV - nreal) // 128] = 0.0
    f32 = lambda a: np.ascontiguousarray(a, dtype=np.float32)
    fm = lambda v: f32(v.reshape(-1, 128).T)
    bc = lambda v: f32(np.broadcast_to(v.reshape(1, -1), (128, v.size)))
    tri = np.triu(np.ones((128, 128), np.float32))
    sel = np.zeros((H, H * 128), np.float32)
    for h in range(H):
        sel[h, h * 128:(h + 1) * 128] = 1.0
    m = {
        "xseq": xseq,
        "w_in": f32(p['w_in'][0]), "w_a": f32(p['w_a'][0]), "w_b": f32(p['w_b'][0]), "w_out": f32(p['w_out'][0]),
        "w_ff1": f32(p['w_ff1'][0]), "w_ff2": f32(p['w_ff2'][0]),
        "g1fm": fm(p['norm1_g'][0]), "g2fm": fm(p['norm2_g'][0]), "gfbc": bc(p['norm_f_g']),
        "lngbc": bc(p['gm_ln_g'][0]), "lnbbc": bc(p['gm_ln_b'][0]), "bsbc": bc(p['gm_bs'][0].reshape(-1)),
        "wsT": f32(np.transpose(p['gm_ws'][0], (2, 0, 1)).reshape(128, G * 128)),
        "tri": tri,
        "cw": f32(np.transpose(p['ml_conv_w'][0].reshape(4, MC, 128), (2, 1, 0)).reshape(128, MC * 4)),
        "cb": fm(p['ml_conv_b'][0]),
        "wq_s": f32(np.transpose(p['ml_wq'][0].reshape(H, DC, 128, Dh), (2, 0, 1, 3)).reshape(128, MC * Dh)),
        "wk_s": f32(np.transpose(p['ml_wk'][0].reshape(H, DC, 128, Dh), (2, 0, 1, 3)).reshape(128, MC * Dh)),
        "igb": f32(p['ml_ig_b'][0].reshape(H, 1)), "fgb": f32(p['ml_fg_b'][0].reshape(H, 1)),
        "ngbc": bc(p['ml_norm_g'][0]),
        "bgate": f32(np.transpose(p['b_gate'][0].reshape(2, KC, 128), (2, 0, 1)).reshape(128, 2 * KC)),
        "mask": mask, "ident": np.eye(128, dtype=np.float32), "sel": sel,
    }
    return m


_NC_CACHE = {}


def kernel(**inputs):
    cfg = REAL_CFG
    p = {k: np.asarray(v) for k, v in inputs.items()}
    n = cfg['NCORES']
    key = tuple(sorted(cfg.items()))
    if key not in _NC_CACHE:
        _NC_CACHE[key] = build(cfg)
    nc = _NC_CACHE[key]
    in_maps = [make_inputs(cfg, p, c) for c in range(n)]
    res = run_bass_kernel_spmd(nc, in_maps, core_ids=list(range(n)))
    Bn, Sn, D = p['x'].shape
    outs = [np.asarray(r["out"], dtype=np.float32) for r in res.results]
    return np.concatenate(outs, axis=0).reshape(Bn, Sn, D)
```

```python
import contextlib
import numpy as np
import concourse.bass as bass
import concourse.mybir as mybir
from concourse.bass_utils import run_bass_kernel_spmd

F32 = mybir.dt.float32
BF16 = mybir.dt.bfloat16
ALU = mybir.AluOpType
AF = mybir.ActivationFunctionType
AX = mybir.AxisListType
EPS = 1e-6
NEG = -1.0e30

REAL_CFG = dict(D=2048, T=1024, NPREV=3072, H=4, FG=2048, NCORES=8)


class Sched:
    def __init__(self):
        self.ops = []
        self.lastw = {}
        self.readers = {}

    def op(self, eng, fn, reads=(), writes=(), dma=None):
        idx = len(self.ops)
        deps = set()
        for k in reads:
            if k in self.lastw:
                deps.add(self.lastw[k])
        for k in writes:
            if k in self.lastw:
                deps.add(self.lastw[k])
            for r in self.readers.get(k, ()):
                deps.add(r)
        deps.discard(idx)
        self.ops.append(dict(eng=eng, fn=fn, deps=deps, dma=dma, marked=False, sig=None))
        for k in reads:
            self.readers.setdefault(k, []).append(idx)
        for k in writes:
            self.lastw[k] = idx
            self.readers[k] = []
        return idx

    def finalize(self):
        ops = self.ops
        dcnt = {}
        for o in ops:
            if o['dma']:
                dcnt[o['dma']] = dcnt.get(o['dma'], 0) + 16
                o['sig'] = (('dma', o['dma']), dcnt[o['dma']])
        for o in ops:
            for d in o['deps']:
                p = ops[d]
                if p['dma']:
                    continue
                if o['eng'] == 'pe' and p['eng'] == 'pe' and not o['dma']:
                    continue
                p['marked'] = True
        cnt = {}
        for o in ops:
            if not o['dma'] and o['marked']:
                cnt[o['eng']] = cnt.get(o['eng'], 0) + 1
                o['sig'] = (('eng', o['eng']), cnt[o['eng']])
        return sorted(dcnt.keys())

    def emit_engine(self, eng, handle, sems):
        ops = self.ops
        seen = {}
        for o in ops:
            if o['eng'] != eng:
                continue
            waits = {}
            for d in o['deps']:
                p = ops[d]
                if (not p['dma']) and eng == 'pe' and p['eng'] == 'pe' and not o['dma']:
                    continue
                sk, val = p['sig']
                if waits.get(sk, 0) < val:
                    waits[sk] = val
            for sk, val in waits.items():
                if seen.get(sk, 0) < val:
                    handle.wait_ge(sems[sk], val)
                    seen[sk] = val
            if o['fn'] is None:
                continue
            ins = o['fn'](handle)
            if o['dma']:
                ins.then_inc(sems[('dma', o['dma'])], 16)
            elif o['marked']:
                ins.then_inc(sems[('eng', eng)], 1)


def build(cfg):
    D = cfg['D']; T = cfg['T']; NPREV = cfg['NPREV']; H = cfg['H']; FG = cfg['FG']
    GW = D // 2; G = GW // 128; Dh = GW // H; DC = Dh // 128; KC = D // 128
    MC = GW // 128; DFF = 4 * D; NIN = 5 * GW + 2 * H + 2 * D
    NT = T // 128; BLK = 512; NBO = T // BLK; NBP = NPREV // BLK; NB = NBP + NBO
    NCHT = (NPREV + T) // 128
    HT = T // 2
    NHT = HT // 128
    KF = FG // 128
    FB = min(512, HT)
    NG = DFF // FG
    SL = 8192
    RS = Dh ** -0.5
    c_u, c_v, c_m, c_mv, c_o = 0, GW, 2 * GW, 3 * GW, 4 * GW
    c_i = 5 * GW; c_f = c_i + H; c_ga = c_f + H; c_gb = c_ga + D

    nc = bass.Bass("TRN2", target_bir_lowering=False, dynamic_dma_scratch_size=8192)
    dt_in = lambda n, s: nc.dram_tensor(n, list(s), F32, kind="ExternalInput").ap()
    xseq = dt_in("xseq", [NPREV + T, D])
    w_in = dt_in("w_in", [D, NIN]); w_a = dt_in("w_a", [GW, D]); w_b = dt_in("w_b", [GW, D])
    w_out = dt_in("w_out", [D, D]); w_ff1 = dt_in("w_ff1", [D, DFF]); w_ff2 = dt_in("w_ff2", [DFF, D])
    g1fm = dt_in("g1fm", [128, KC]); g2fm = dt_in("g2fm", [128, KC]); gfbc = dt_in("gfbc", [128, D])
    lngbc = dt_in("lngbc", [128, GW]); lnbbc = dt_in("lnbbc", [128, GW]); bsbc = dt_in("bsbc", [128, GW])
    wsT_d = dt_in("wsT", [128, GW]); tri_d = dt_in("tri", [128, 128])
    cw_d = dt_in("cw", [128, MC * 4]); cb_d = dt_in("cb", [128, MC])
    wq_d = dt_in("wq_s", [128, MC * Dh]); wk_d = dt_in("wk_s", [128, MC * Dh])
    igb_d = dt_in("igb", [H, 1]); fgb_d = dt_in("fgb", [H, 1]); ngbc_d = dt_in("ngbc", [128, GW])
    bgate_d = dt_in("bgate", [128, 2 * KC]); mask_d = dt_in("mask", [H, NCHT])
    ident_d = dt_in("ident", [128, 128]); sel_d = dt_in("sel", [H, H * 128])
    out_d = nc.dram_tensor("out", [T, D], F32, kind="ExternalOutput").ap()

    S = Sched()
    es = contextlib.ExitStack()

    def sb(name, shape, dtype):
        return es.enter_context(nc.sbuf_tensor(name, list(shape), dtype))

    def ps(name, shape, dtype):
        return es.enter_context(nc.psum_tensor(name, list(shape), dtype))

    ROWS_N = 12 * 128
    A_P1 = D + ROWS_N + H * 128 + D // 2 + 2 * 512 + (KC * 512 + MC * 512 + 4 * GW + MC * 512) // 2
    A_N = max(NT * D, A_P1)
    B_N = max(KC * T, NT * GW + 2 * (MC * Dh + 2 * H * 128 + 2 * (Dh + 4)) + H * 128)
    A = sb("A", [128, A_N], F32)
    B = sb("B", [128, B_N], BF16)
    C = sb("C", [128, 16384], BF16)
    RING = sb("RING", [128, 4 * SL], BF16)
    PCN = sb("PCN", [128, 3 * GW], F32)
    A_bf = A[:].bitcast(BF16)
    B_f = B[:].bitcast(F32)
    C_f = C[:].bitcast(F32)

    def vbf(base, off, shape):
        n = int(np.prod(shape))
        v = base[:, off:off + n]
        if len(shape) == 2:
            return v.rearrange("p (a b) -> p a b", a=shape[0])
        if len(shape) == 3:
            return v.rearrange("p (a b c) -> p a b c", a=shape[0], b=shape[1])
        return v

    of = 0
    xst = [A[:, 0:D], A[:, 0:D]]; of = D
    rows = A[0:H, of:of + ROWS_N]; of += ROWS_N
    sel = A[0:H, of:of + H * 128]; of += H * 128
    xs = [A_bf[:, 2 * of:2 * of + D], A_bf[:, 2 * of:2 * of + D]]; of += D // 2
    gblk = A[0:H, of:of + 2 * 512]; of += 2 * 512
    o = 2 * of
    xnTb = vbf(A_bf, o, [KC, BLK]); o += KC * BLK
    cT = vbf(A_bf, o, [MC, BLK]); o += MC * BLK
    kw = vbf(A_bf, o, [4, H, Dh]); o += 4 * GW
    qT = vbf(A_bf, o, [MC, BLK]); o += MC * BLK
    assert o <= 2 * A_N, (o, A_N)
    xnT = vbf(A_bf, 0, [KC, T])
    yaT = vbf(A_bf, KC * T, [G, T])
    ybT = vbf(A_bf, KC * T + G * T, [MC, T])
    x1 = A[:, 0:NHT * D].rearrange("p (a b) -> p a b", a=NHT)
    hnT = vbf(A_bf, 2 * NHT * D, [KC, HT])
    h1T = vbf(A_bf, 2 * NHT * D + KC * HT, [KF, HT])
    assert 2 * NHT * D + KC * HT + KF * HT <= 2 * A_N
    hn_store = vbf(B[:], 0, [NT, GW])
    v_ln = vbf(B[:], NT * GW, [NT, GW])
    mixedT = vbf(B[:], 0, [KC, T])
    ob = NT * GW // 2
    Cst = vbf(B_f, ob, [MC, Dh]); ob += MC * Dh
    Ebuf = vbf(B_f, ob, [H, 128]); ob += H * 128
    zt = vbf(B_f, ob, [H, 128]); ob += H * 128
    numden = B_f[:, ob:ob + Dh + 4]; ob += Dh + 4
    intra_sb = B_f[:, ob:ob + Dh + 4]; ob += Dh + 4
    PT = vbf(B[:], 2 * ob, [H, 128]); ob += H * 64
    assert ob <= B_N // 2, (ob, B_N)
    oc = 0
    VW = Dh + 2
    vsb = vbf(C[:], oc, [4, H, VW]); oc += 4 * H * VW
    kT = vbf(C[:], oc, [MC, BLK]); oc += MC * BLK
    Cb = vbf(C[:], oc, [MC, VW]); oc += MC * VW
    hbuf = C_f[:, oc // 2:oc // 2 + GW]; oc += 2 * GW
    mbuf = [C_f[:, oc // 2 + i * 520:oc // 2 + i * 520 + 520] for i in range(2)]; oc += 2 * 1040
    tacc = C_f[:, oc // 2:oc // 2 + BLK]; oc += 2 * BLK
    assert oc <= 16384, oc
    gx = [C_f[:, i * 512:(i + 1) * 512] for i in range(2)]
    gt = [C_f[:, 1024 + i * 512:1024 + (i + 1) * 512] for i in range(2)]
    gz = [C_f[:, 2048 + i * 512:2048 + (i + 1) * 512] for i in range(2)]
    gv = C_f[:, 3072:3072 + GW]
    tm1 = C_f[:, 3072 + GW:3072 + 2 * GW]
    ybtm = C[:, 2 * (3072 + 2 * GW):2 * (3072 + 2 * GW) + 512]
    assert 3072 + 2 * GW + 256 <= 8192
    gfb = C_f[:, 0:D]
    ost = [C_f[:, D + i * D:D + (i + 1) * D] for i in range(2)]
    assert 3 * D <= 8192 or D < 2048
    wk_s = vbf(PCN[:].bitcast(BF16), 0, [MC, Dh])
    wq_s = vbf(PCN[:].bitcast(BF16), MC * Dh, [MC, Dh])
    XST1_OK = (3 * GW - MC * Dh // 2) >= D and (MC * Dh // 2) <= GW and 2 * GW >= D
    xst1 = PCN[:, 3 * GW - D:3 * GW] if XST1_OK else None
    assert MC * Dh <= 2 * GW
    ngbc = PCN[:, 2 * GW:3 * GW]
    xs1 = hbuf.bitcast(BF16)[:, 0:D] if 2 * GW >= D else None
    lng_s = PCN[:, 0:GW]; lnb_s = PCN[:, GW:2 * GW]; bs_s = PCN[:, 2 * GW:3 * GW]

    g1s = sb("g1s", [128, KC], F32); g2s = sb("g2s", [128, KC], F32)
    wsTm = sb("wsTm", [128, GW], BF16)
    wsT = C[:, 0:GW]
    tri = sb("tri_s", [128, 128], F32)
    cw = sb("cw_s", [128, MC * 4], F32); cb = sb("cb_s", [128, MC], F32)
    igb = sb("igb_s", [H, 1], F32); fgb = sb("fgb_s", [H, 1], F32); nfgb = sb("nfgb", [H, 1], F32)
    bgate = sb("bgate_s", [128, 2 * KC], F32); mask = sb("mask_s", [H, NCHT], F32)
    identb = sb("identb", [128, 128], BF16); identf = sb("identf", [128, 128], F32)
    onesH = sb("onesH", [H, 128], F32)
    onesb = sb("onesb", [128, 1], BF16)
    wg = sb("wg", [128, KC * 2 * H], BF16)
    halo = sb("halo", [128, MC * 4], F32)
    nst = sb("nst", [128, MC], F32)
    stat = sb("stat", [128, 64], F32)
    junkF = C[:, 12288:12288 + D]
    rsm = sb("rsm", [H, 64], F32)
    cols = sb("cols", [128, 4 * H], F32)
    dsb = sb("dsb", [128, 2 * H], F32)
    bnst = sb("bnst", [128, 4 * 6], F32)
    dummy = sb("dummy_t", [128, 4], F32)

    PB = [ps("pb%d" % i, [128, 512], F32) for i in range(8)]

    def pbf(i):
        return PB[i][:].bitcast(BF16)

    REGKEYS = {}

    def K(region, *rest):
        return (region,) + rest

    def op(eng, fn, reads=(), writes=(), dma=None):
        reads = list(reads); writes = list(writes)
        regs = set()
        for k in reads + writes:
            if k[0] in REGKEYS:
                regs.add(('REGION', k[0]))
                REGKEYS[k[0]].add(k)
        return S.op(eng, fn, reads + list(regs), writes, dma)

    for r in ('A', 'B', 'C', 'PCN'):
        REGKEYS[r] = set()

    def region_barrier(region):
        old = list(REGKEYS[region])
        REGKEYS[region] = set()
        S.op('dve', lambda e: e.memset(dummy[:, 0:1], 0.0), reads=(), writes=old + [('REGION', region), ('dummy',)])

    def dma_sp(out, in_, reads, writes, sem):
        op('sp', lambda e: e.dma_start(out=out, in_=in_), reads, writes, dma=sem)

    def dma_cast(out, in_, reads, writes, sem):
        op('pool', lambda e: e.dma_start(out=out, in_=in_), reads, writes, dma=sem)

    CONST = ('consts',)

    for (dst, src) in [(g1s, g1fm), (g2s, g2fm), (tri, tri_d), (cw, cw_d), (cb, cb_d),
                       (igb, igb_d), (fgb, fgb_d), (bgate, bgate_d), (mask, mask_d), (identf, ident_d)]:
        dma_sp(dst[:], src, [], [CONST], 'const')
    dma_cast(identb[:], ident_d, [], [CONST], 'constc')
    dma_cast(wsT, wsT_d, [], [('C', 'vsb')], 'wsld')
    dma_sp(sel, sel_d, [], [('A', 'sel')], 'selc')
    dma_cast(wg[:].rearrange("p (k n) -> p k n", k=KC),
             w_in.rearrange("(k p) n -> p k n", p=128)[:, :, c_i:c_i + 2 * H], [], [CONST], 'constc')

    def init_consts(e):
        e.memset(onesH[:], 1.0)
        e.memset(nhalf[:], -0.5)
        e.memset(onesb[:], 1.0)
        e.memset(halo[:], 0.0)
        e.memset(nst[:], 0.0)
        e.memset(rsm[:], 0.0)
        e.tensor_scalar(out=nfgb[:], in0=fgb[:], scalar1=-1.0, scalar2=None, op0=ALU.mult)
        e.tensor_scalar(out=nbm[:], in0=mask[:], scalar1=-1.0, scalar2=1.0e30, op0=ALU.add, op1=ALU.mult)
        return e.tensor_tensor(out=wsTm[:].rearrange("p (g t) -> p g t", g=G),
                               in0=wsT.rearrange("p (g t) -> p g t", g=G),
                               in1=tri[:].unsqueeze(1).broadcast_to([128, G, 128]), op=ALU.mult)
    op('dve', init_consts, [CONST, ('C', 'vsb')], [('init',)])
    INIT = [CONST, ('init',)]

    ring_state = dict(n=0)

    def slab_load(src_ap, kc, cbw):
        slot = ring_state['n'] % 4
        ring_state['n'] += 1
        view = RING[:, slot * SL:slot * SL + kc * cbw].rearrange("p (k n) -> p k n", k=kc)
        key = ('slab', slot)
        srcv = src_ap.rearrange("(k p) n -> p k n", p=128)
        dma_cast(view, srcv, [], [key], 'slab%d' % slot)
        return view, key, ring_state['n'] - 1

    def mm_fm(bank_ap, slab, col0, act, t0, nt, kc, reads, writes):
        def fn(e):
            ins = None
            for k in range(kc):
                ins = e.matmul(bank_ap, lhsT=slab[:, k, col0:col0 + 128], rhs=act[:, k, t0:t0 + nt],
                               start=(k == 0), stop=(k == kc - 1))
            return ins
        op('pe', fn, reads, writes)

    def mm_tm(bank_ap, act, t0, slab, c0, cw_, kc, reads, writes):
        def fn(e):
            ins = None
            for k in range(kc):
                ins = e.matmul(bank_ap, lhsT=act[:, k, t0:t0 + 128], rhs=slab[:, k, c0:c0 + cw_],
                               start=(k == 0), stop=(k == kc - 1))
            return ins
        op('pe', fn, reads, writes)

    PSK = lambda b: ('ps', b)

    def norm_transpose(src_tile_ap, src_key, gfm, dstT, dst_t0, dst_keys, xi, sidx, xreg='A', use_pool=False, defer_back=False):
        ss = stat[:, sidx:sidx + 1]; rs = stat[:, sidx + 1:sidx + 2]
        sk = ('stat', sidx)
        xk = K(xreg, 'xs', xi)
        xs_ap = xs[xi]
        op('act', lambda e: e.activation(out=xs_ap, in_=src_tile_ap, func=AF.Square, accum_out=ss),
           [src_key], [xk, sk])

        def rstd_fn(e):
            return e.tensor_scalar(out=rs, in0=ss, scalar1=1.0 / D, scalar2=EPS, op0=ALU.mult, op1=ALU.add)
        op('dve', rstd_fn, [sk], [('stat', sidx + 1)])
        if use_pool:
            op('pool', lambda e: e.tensor_tensor(out=rs, in0=rs, in1=nhalf[:, 0:1], op=ALU.pow), [('stat', sidx + 1)] + INIT,
               [('stat', sidx + 1)])
            op('pool', lambda e: e.tensor_scalar(out=xs_ap, in0=src_tile_ap, scalar1=rs, scalar2=1.0, op0=ALU.mult, op1=ALU.mult),
               [src_key, ('stat', sidx + 1)], [xk])
        else:
            op('act', lambda e: e.activation(out=rs, in_=rs, func=AF.Sqrt), [('stat', sidx + 1)], [('stat', sidx + 1)])
            op('dve', lambda e: e.reciprocal(out=rs, in_=rs), [('stat', sidx + 1)], [('stat', sidx + 1)])
            op('act', lambda e: e.activation(out=xs_ap, in_=src_tile_ap, func=AF.Copy, scale=rs),
               [src_key, ('stat', sidx + 1)], [xk])
        nb = (KC * 128 + 1023) // 1024
        def back():
            def tr_fn(e):
                ins = None
                for k in range(KC):
                    bank = k // 8
                    ins = e.transpose(out=pbf(bank)[:, (k % 8) * 128:(k % 8 + 1) * 128],
                                      in_=xs_ap[:, k * 128:(k + 1) * 128], identity=identb[:])
                return ins
            op('pe', tr_fn, [xk] + INIT, [PSK(b) for b in range(nb)])
            for b in range(nb):
                k0 = b * 8; k1 = min(KC, k0 + 8)
                def ev_fn(e, b=b, k0=k0, k1=k1):
                    return e.tensor_tensor(
                        out=dstT[:, k0:k1, dst_t0:dst_t0 + 128],
                        in0=pbf(b)[:, 0:(k1 - k0) * 128].rearrange("p (k t) -> p k t", k=k1 - k0),
                        in1=gfm[:, k0:k1].unsqueeze(2).broadcast_to([128, k1 - k0, 128]), op=ALU.mult)
                op('dve', ev_fn, [PSK(b)] + INIT, dst_keys)
        if defer_back:
            return back
        back()
        return None

    dma_cast(wk_s.rearrange("p a b -> p (a b)"), wk_d, [], [K('PCN', 'wk')], 'pck')
    wsrc = w_in
    slab_m = []; slab_mv = []
    for j in range(max(1, GW // 512)):
        cbw = min(512, GW)
        slab_m.append(slab_load(wsrc[:, c_m + j * cbw:c_m + (j + 1) * cbw], KC, cbw)[0:2])
    for j in range(max(1, GW // 512)):
        cbw = min(512, GW)
        slab_mv.append(slab_load(wsrc[:, c_mv + j * cbw:c_mv + (j + 1) * cbw], KC, cbw)[0:2])
    CBW = min(512, GW)
    CPS = CBW // 128

    op('dve', lambda e: e.memset(Cst.rearrange("p a b -> p (a b)"), 0.0), [], [K('B', 'Cst', hh) for hh in range(H)])
    def vinit(e):
        e.memset(vsb.rearrange("p a b c -> p (a b c)"), 1.0)
        return e.memset(Cb.rearrange("p a b -> p (a b)"), 0.0)
    op('dve', vinit, [], [K('C', 'vsb'), K('C', 'Cb')])

    R = lambda i: rows[:, i * 128:(i + 1) * 128]
    m_slot = [rsm[:, 0:1], rsm[:, 1:2]]
    xdma = dict(n=0)


    cols4 = sb("cols4", [128, 4 * 4 * H], F32)
    dsb4 = sb("dsb4", [128, 4 * 2 * H], F32)
    Mr4 = sb("Mr4", [H, 4 * 128], F32)
    tacc2 = sb("tacc2", [128, BLK], F32)
    ksb = sb("ksb", [128, MC], F32)
    nbm = sb("nbm", [H, NCHT], F32)
    nhalf = sb("nhalf", [128, 1], F32)
    dgt = sb("dgt", [H, 4 * 2 * H], F32)
    taccs = [tacc, tacc2[:]]
    fmb = dict(n=0)
    FMB = [2, 6, 7]

    DBL = XST1_OK and xs1 is not None
    def s1_tile(b, i, defer=False):
        r0 = b * BLK + i * 128
        par = (i % 2) if (DBL and b < NBP) else 0
        if par == 0:
            xkey = K('A', 'xst', 0)
            dma_sp(xst[0], xseq[r0:r0 + 128, :], [], [xkey], 'xst0')
            xs[0] = xs0_p1
            return norm_transpose(xst[0], xkey, g1s, xnTb, i * 128, [K('A', 'xnTb')], 0, 0, use_pool=True, defer_back=defer)
        else:
            xkey = K('PCN', 'xst', 1)
            dma_sp(xst1, xseq[r0:r0 + 128, :], [], [xkey], 'xst1')
            xs[1] = xs1
            return norm_transpose(xst1, xkey, g1s, xnTb, i * 128, [K('A', 'xnTb')], 1, 2, xreg='C', use_pool=True, defer_back=defer)

    xs0_p1 = xs[0]

    def p1_block(b):
        own = b >= NBP
        ob_ = b - NBP
        XB = K('A', 'xnTb')
        if b == NBP:
            dma_cast(wq_s.rearrange("p a b -> p (a b)"), wq_d, [K('PCN', 'xst', 1)], [K('PCN', 'wq'), K('PCN', 'xst', 1)], 'pcq')
            dma_sp(ngbc, ngbc_d, [], [K('PCN', 'ng'), K('PCN', 'xst', 1)], 'pcg')
        if b == 0:
            for i in range(4):
                s1_tile(b, i)
        def gate_fn(e):
            ins = None
            for gi in range(2):
                for k in range(KC):
                    ins = e.matmul(PB[3 + gi][0:H, 0:BLK], lhsT=wg[:, k * 2 * H + gi * H:k * 2 * H + (gi + 1) * H],
                                   rhs=xnTb[:, k, :], start=(k == 0), stop=(k == KC - 1))
            return ins
        op('pe', gate_fn, [XB] + INIT, [PSK(3), PSK(4)])
        def gcp(e):
            e.activation(out=gblk[:, 0:BLK], in_=PB[3][0:H, 0:BLK], func=AF.Identity, bias=igb[:])
            return e.activation(out=gblk[:, BLK:2 * BLK], in_=PB[4][0:H, 0:BLK], func=AF.Exp, scale=-1.0, bias=nfgb[:])
        op('act', gcp, [PSK(3), PSK(4)] + INIT, [('A', 'gi'), ('A', 'gf')])

        def s3(j):
            sl, skey = slab_m[j // CPS]
            bk = FMB[fmb['n'] % 3]; fmb['n'] += 1
            mm_fm(PB[bk][:, 0:BLK], sl, (j % CPS) * 128, xnTb, 0, BLK, KC, [XB, skey], [PSK(bk)])
            mb = mbuf[j % 2]; mk = K('C', 'mbuf', j % 2)
            def mev(e):
                e.copy(out=mb[:, 0:3], in_=halo[:, j * 4:j * 4 + 3])
                return e.copy(out=mb[:, 3:3 + BLK], in_=PB[bk][:, 0:BLK])
            op('act', mev, [PSK(bk), ('halo', j)] + INIT, [mk])
            op('act', lambda e: e.copy(out=halo[:, j * 4:j * 4 + 3], in_=PB[bk][:, BLK - 3:BLK]), [PSK(bk)], [('halo', j)])
            ta = taccs[j % 2]; tk = ('tacc', j % 2)
            op('act', lambda e: e.activation(out=ta, in_=mb[:, 0:BLK], func=AF.Identity, scale=cw[:, j * 4:j * 4 + 1],
                                             bias=cb[:, j:j + 1]), [mk] + INIT, [tk])
            for tap in range(1, 4):
                op('dve', lambda e, tap=tap: e.scalar_tensor_tensor(
                    out=ta, in0=mb[:, tap:tap + BLK], scalar=cw[:, j * 4 + tap:j * 4 + tap + 1], in1=ta,
                    op0=ALU.mult, op1=ALU.add), [mk, tk] + INIT, [tk])
            return lambda: op('act', lambda e: e.activation(out=cT[:, j, :], in_=ta, func=AF.Silu), [tk], [K('A', 'cT', j)])
        CTK = [K('A', 'cT', j) for j in range(MC)]

        def GB():
            gi = gblk[:, 0:BLK]; gf = gblk[:, BLK:2 * BLK]
            bcum = rows[:, 0:BLK]; gg = rows[:, BLK:2 * BLK]; Gc = rows[:, 2 * BLK:3 * BLK]
            v3 = lambda ap: ap.rearrange("p (c t) -> p c t", c=4)
            P_ = b % 2
            mm = rsm[:, 16 + 8 * P_:16 + 8 * P_ + 5]; mmo = rsm[:, 16 + 8 * (1 - P_):16 + 8 * (1 - P_) + 5]
            mprev = mm[:, 0:4]; mnew = mm[:, 1:5]
            mk4 = mask[:, b * 4:(b + 1) * 4]; nb4 = nbm[:, b * 4:(b + 1) * 4]
            bl = bcum[:, 127:BLK:128]; Gl = Gc[:, 127:BLK:128]
            am = rsm[:, 32:36]; blp = rsm[:, 36:40]; amp = rsm[:, 40:44]; d12 = rsm[:, 44:52]; ds = rsm[:, 52:60]
            KQ = lambda n: ('gq', n)
            op('act', lambda e: e.activation(out=gf, in_=gf, func=AF.Ln, bias=1.0), [('A', 'gf')], [('A', 'gf')])
            yield
            def scan1(e):
                ins = None
                for c in range(4):
                    ins = e.tensor_tensor_scan(out=bcum[:, c * 128:(c + 1) * 128], data0=onesH[:], data1=gf[:, c * 128:(c + 1) * 128],
                                               initial=0.0, op0=ALU.mult, op1=ALU.subtract)
                return ins
            op('dve', scan1, [('A', 'gf')] + INIT, [('A', 'bcum')])
            op('dve', lambda e: e.tensor_tensor(out=gg, in0=gi, in1=bcum, op=ALU.subtract), [('A', 'gi'), ('A', 'bcum')], [('A', 'gg')])
            yield
            def scan2(e):
                ins = None
                for c in range(4):
                    ins = e.tensor_tensor_scan(out=Gc[:, c * 128:(c + 1) * 128], data0=gg[:, c * 128:(c + 1) * 128],
                                               data1=gg[:, c * 128:(c + 1) * 128], initial=NEG, op0=ALU.max, op1=ALU.max)
                return ins
            op('dve', scan2, [('A', 'gg')], [('A', 'Gc')])
            def sca(e):
                e.tensor_tensor(out=am, in0=bl, in1=Gl, op=ALU.add)
                return e.tensor_tensor(out=blp, in0=bl, in1=mk4, op=ALU.mult)
            op('dve', sca, [('A', 'bcum'), ('A', 'Gc')] + INIT, [KQ('am'), KQ('blp')])
            yield
            def scb(e):
                e.tensor_tensor(out=amp, in0=am, in1=mk4, op=ALU.mult)
                return e.tensor_tensor(out=blp, in0=blp, in1=nb4, op=ALU.add)
            op('dve', scb, [KQ('am'), KQ('blp')] + INIT, [KQ('amp'), KQ('blp')])
            if b > 0:
                op('dve', lambda e: e.tensor_copy(out=mm[:, 0:1], in_=mmo[:, 4:5]), [('mm', 1 - P_)], [('mm0', P_)])
            op('dve', lambda e: e.tensor_tensor_scan(out=mnew, data0=blp, data1=amp, initial=mm[:, 0:1], op0=ALU.add, op1=ALU.max),
               [KQ('amp'), KQ('blp'), ('mm0', P_)] + INIT, [('mm', P_)])
            yield
            def scc(e):
                e.tensor_tensor(out=d12[:, 0:4], in0=blp, in1=mprev, op=ALU.add)
                return e.tensor_tensor(out=d12[:, 4:8], in0=amp, in1=mnew, op=ALU.subtract)
            op('dve', scc, [KQ('amp'), KQ('blp'), ('mm', P_), ('mm0', P_)] + INIT, [KQ('d12a')])
            op('dve', lambda e: e.tensor_tensor(out=d12[:, 0:4], in0=d12[:, 0:4], in1=mnew, op=ALU.subtract),
               [KQ('d12a'), ('mm', P_)], [KQ('d12')])
            op('act', lambda e: e.activation(out=ds, in_=d12, func=AF.Exp), [KQ('d12'), KQ('d12a')], [KQ('ds')])
            yield
            op('dve', lambda e: e.tensor_tensor(out=ds[:, 4:8], in0=ds[:, 4:8], in1=mk4, op=ALU.mult), [KQ('ds')] + INIT, [KQ('ds2')])
            dg3 = dgt[:].rearrange("p (c n) -> p c n", c=4)
            idb = identf[0:H, 0:H].unsqueeze(1).broadcast_to([H, 4, H])
            def scd(e):
                e.tensor_tensor(out=dg3[:, :, 0:H], in0=idb, in1=ds[:, 0:4].unsqueeze(2).broadcast_to([H, 4, H]), op=ALU.mult)
                return e.tensor_tensor(out=dg3[:, :, H:2 * H], in0=idb, in1=ds[:, 4:8].unsqueeze(2).broadcast_to([H, 4, H]),
                                       op=ALU.mult)
            op('dve', scd, [KQ('ds'), KQ('ds2')] + INIT, [KQ('dgt')])
            op('dve', lambda e: e.tensor_tensor(out=v3(gi), in0=v3(gg), in1=Gl.unsqueeze(2).broadcast_to([H, 4, 128]),
                                                op=ALU.subtract), [('A', 'gg'), ('A', 'Gc'), ('A', 'gi')], [('A', 'gi')])
            op('act', lambda e: e.activation(out=gi, in_=gi, func=AF.Exp), [('A', 'gi')], [('A', 'wa')])
            yield
            if own:
                mpb = mprev.unsqueeze(2).broadcast_to([H, 4, 128])
                op('dve', lambda e: e.tensor_tensor(out=v3(Mr4[:]), in0=v3(Gc), in1=mpb, op=ALU.max),
                   [('A', 'Gc'), ('mm', P_), ('mm0', P_)], [('Mr',)])
                op('dve', lambda e: e.tensor_tensor(out=v3(gf), in0=mpb, in1=v3(Mr4[:]), op=ALU.subtract),
                   [('Mr',), ('mm', P_), ('mm0', P_), ('A', 'gf'), ('A', 'bcum')], [('A', 'gf')])
                op('dve', lambda e: e.tensor_tensor(out=bcum, in0=bcum, in1=Mr4[:], op=ALU.add),
                   [('Mr',), ('A', 'bcum'), KQ('am'), KQ('blp')], [('A', 'bcum')])
                def ex2(e):
                    e.activation(out=gf, in_=gf, func=AF.Exp)
                    return e.activation(out=bcum, in_=bcum, func=AF.Exp, scale=-1.0)
                op('act', ex2, [('A', 'gf'), ('A', 'bcum')], [('A', 'winter'), ('A', 'emt')])
            yield 'pe'
            def bc_fn(e):
                ins = e.matmul(PB[5][:, 0:8 * H], lhsT=onesH[:], rhs=dgt[:], start=True, stop=True)
                for c in range(4):
                    cs_ = slice(c * 128, (c + 1) * 128); c0 = 64 + c * 4 * H
                    ins = e.transpose(out=PB[5][:, c0:c0 + H], in_=gi[:, cs_], identity=identf[0:H, 0:H])
                    if own:
                        e.transpose(out=PB[5][:, c0 + H:c0 + 2 * H], in_=gg[:, cs_], identity=identf[0:H, 0:H])
                        e.transpose(out=PB[5][:, c0 + 2 * H:c0 + 3 * H], in_=gf[:, cs_], identity=identf[0:H, 0:H])
                        ins = e.transpose(out=PB[5][:, c0 + 3 * H:c0 + 4 * H], in_=bcum[:, cs_], identity=identf[0:H, 0:H])
                return ins
            op('pe', bc_fn, [KQ('dgt'), ('A', 'wa'), ('A', 'gg'), ('A', 'winter'), ('A', 'emt')] + INIT, [PSK(5)])
            ncol = 4 * H if own else H
            def bc_ev(e):
                e.tensor_copy(out=dsb4[:, 0:8 * H], in_=PB[5][:, 0:8 * H])
                return e.tensor_copy(out=cols4[:].rearrange("p (c n) -> p c n", c=4)[:, :, 0:ncol],
                                     in_=PB[5][:, 64:64 + 16 * H].rearrange("p (c n) -> p c n", c=4)[:, :, 0:ncol])
            op('dve', bc_ev, [PSK(5)], [('dsb', i_) for i_ in range(4)] + [('cols', i_) for i_ in range(4)])

        gbg = GB()
        gb_state = dict(at_pe=False, done=False)
        def gb_step(allow_pe=False):
            if gb_state['done'] or (gb_state['at_pe'] and not allow_pe):
                return False
            try:
                r = next(gbg)
                if r == 'pe':
                    gb_state['at_pe'] = True
                return True
            except StopIteration:
                gb_state['done'] = True
                return False
        pend = None
        for j in range(MC):
            nxt = s3(j)
            if pend is not None:
                pend()
            pend = nxt
        pend()

        if own:
            for (wsx, dst, dkey, scale) in ((wq_s, qT, 'qT', 1.0), (wk_s, kT, 'kT', RS)):
                for hh in range(H):
                    for ec in range(DC):
                        bk = FMB[fmb['n'] % 3]; fmb['n'] += 1
                        def fn(e, wsx=wsx, hh=hh, ec=ec, bk=bk):
                            ins = None
                            for dc in range(DC):
                                ins = e.matmul(PB[bk][:, 0:BLK], lhsT=wsx[:, hh * DC + dc, ec * 128:(ec + 1) * 128],
                                               rhs=cT[:, hh * DC + dc, :], start=(dc == 0), stop=(dc == DC - 1))
                            return ins
                        op('pe', fn, CTK + [K('PCN', 'wq'), K('PCN', 'wk')], [PSK(bk)])
                        reg = 'A' if dkey == 'qT' else 'C'
                        op('act', lambda e, dst=dst, hh=hh, ec=ec, scale=scale, bk=bk: e.activation(
                            out=dst[:, hh * DC + ec, :], in_=PB[bk][:, 0:BLK], func=AF.Copy, scale=scale),
                           [PSK(bk)], [K(reg, dkey, hh * DC + ec)])

        def V(i):
            for j in range(len(slab_mv)):
                sl, skey = slab_mv[j]
                bk = [6, 7][j % 2]
                mm_tm(PB[bk][:, 0:CBW], xnTb, i * 128, sl, 0, CBW, KC, [XB, skey], [PSK(bk)])
                nh = CBW // Dh
                op('act', lambda e, j=j, nh=nh, bk=bk: e.copy(out=vsb[:, i, j * nh:(j + 1) * nh, 0:Dh],
                                                        in_=PB[bk][:, 0:CBW].rearrange("p (h d) -> p h d", h=nh)),
                   [PSK(bk)], [K('C', 'vsb', i, j)])
        for i in range(4):
            V(i)
            gb_step(); gb_step()
        while gb_step(allow_pe=True):
            pass

        def KOU(i):
            ti = ob_ * 4 + i
            csl = slice(i * 128, (i + 1) * 128)
            colsI = cols4[:, i * 4 * H:(i + 1) * 4 * H]
            dsbI = dsb4[:, i * 2 * H:(i + 1) * 2 * H]
            Mr = Mr4[:, i * 128:(i + 1) * 128]
            CK = ('cols', i); DK = ('dsb', i)
            for hp in range(0, H, 2):
                bk = [2, 6][(hp // 2) % 2]
                def kfn(e, hp=hp, bk=bk):
                    ins = None
                    for hh in range(hp, hp + 2):
                        for dc in range(DC):
                            ins = e.matmul(PB[bk][:, (hh - hp) * Dh:(hh - hp + 1) * Dh],
                                           lhsT=cT[:, hh * DC + dc, csl], rhs=wk_s[:, hh * DC + dc, :],
                                           start=(dc == 0), stop=(dc == DC - 1))
                    return ins
                op('pe', kfn, CTK + [K('PCN', 'wk')], [PSK(bk)])
                def kev(e, hp=hp, bk=bk):
                    ins = None
                    for hh in range(hp, hp + 2):
                        ins = e.tensor_scalar(out=kw[:, i, hh, :], in0=PB[bk][:, (hh - hp) * Dh:(hh - hp + 1) * Dh],
                                              scalar1=colsI[:, hh:hh + 1], scalar2=RS, op0=ALU.mult, op1=ALU.mult)
                    return ins
                op('dve', kev, [PSK(bk), CK], [K('A', 'kw', i, hp)])
            VK = [K('C', 'vsb', i, j) for j in range(len(slab_mv))] + [K('C', 'vsb')]
            KWK = [K('A', 'kw', i, hp) for hp in range(0, H, 2)]
            if own:
                def cbf(e):
                    e.copy(out=Cb[:, :, 0:Dh], in_=Cst)
                    return e.copy(out=Cb[:, :, Dh:Dh + 1], in_=nst[:].unsqueeze(2))
                op('act', cbf, [K('B', 'Cst', hh) for hh in range(H)] + [('nst', hh) for hh in range(H)] + INIT, [K('C', 'Cb')])
                tcs = csl
                def sfn(e):
                    ins = None
                    for hh in range(H):
                        for dc in range(DC):
                            ins = e.matmul(PB[3][:, hh * 128:(hh + 1) * 128], lhsT=kT[:, hh * DC + dc, tcs],
                                           rhs=qT[:, hh * DC + dc, tcs], start=(dc == 0), stop=(dc == DC - 1))
                    for hh in range(H):
                        ins = e.matmul(PB[4][:, hh * 128:(hh + 1) * 128], lhsT=sel[:, hh * 128:(hh + 1) * 128],
                                       rhs=Mr, start=True, stop=True)
                    return ins
                op('pe', sfn, [K('A', 'qT', c) for c in range(MC)] + [K('C', 'kT', c) for c in range(MC)]
                   + [('Mr',), ('A', 'sel')] + INIT, [PSK(3), PSK(4)])
                def zfn(e):
                    ins = None
                    for hh in range(H):
                        ins = e.tensor_scalar(out=zt[:, hh, :], in0=PB[4][:, hh * 128:(hh + 1) * 128],
                                              scalar1=colsI[:, H + hh:H + hh + 1], scalar2=None, op0=ALU.max)
                    return ins
                op('dve', zfn, [PSK(4), CK], [K('B', 'zt')])
                def efn(e):
                    ins = None
                    for hh in range(H):
                        ins = e.activation(out=Ebuf[:, hh, :], in_=zt[:, hh, :], func=AF.Exp, scale=-1.0,
                                           bias=colsI[:, H + hh:H + hh + 1])
                    return ins
                op('act', efn, [K('B', 'zt'), CK], [K('B', 'E')])
                op('dve', lambda e: e.tensor_tensor(out=Ebuf, in0=Ebuf,
                                                    in1=tri[:].unsqueeze(1).broadcast_to([128, H, 128]), op=ALU.mult),
                   [K('B', 'E')] + INIT, [K('B', 'E')])
                op('dve', lambda e: e.tensor_tensor(out=PT.rearrange("p a b -> p (a b)"),
                                                    in0=Ebuf.rearrange("p a b -> p (a b)"), in1=PB[3][:, 0:H * 128],
                                                    op=ALU.mult),
                   [K('B', 'E'), PSK(3)], [K('B', 'PT')])
                dent = stat[:, 24:24 + H]; dt2 = stat[:, 32:32 + H]; rden = stat[:, 40:40 + H]
                mvt = bnst[:, 0:0]
                IB = [6, 2]; EB = [7, 1]
                for hh in range(H):
                    def nfn(e, hh=hh):
                        e.matmul(PB[IB[hh % 2]][:, 0:Dh + 1], lhsT=PT[:, hh, :], rhs=vsb[:, i, hh, 0:Dh + 1], start=True, stop=True)
                        ins = None
                        for dc in range(DC):
                            ins = e.matmul(PB[EB[hh % 2]][:, 0:Dh + 1], lhsT=qT[:, hh * DC + dc, tcs],
                                           rhs=Cb[:, hh * DC + dc, 0:Dh + 1], start=(dc == 0), stop=(dc == DC - 1))
                        return ins
                    op('pe', nfn, [K('B', 'PT'), K('C', 'Cb')] + VK + [K('A', 'qT', c) for c in range(MC)],
                       [PSK(IB[hh % 2]), PSK(EB[hh % 2])])
                    hsl = slice(hh * Dh, (hh + 1) * Dh)
                    def icp(e, hh=hh, hsl=hsl):
                        e.copy(out=hbuf[:, hsl], in_=PB[IB[hh % 2]][:, 0:Dh])
                        return e.copy(out=dt2[:, hh:hh + 1], in_=PB[IB[hh % 2]][:, Dh:Dh + 1])
                    op('act', icp, [PSK(IB[hh % 2])], [K('C', 'hbuf', hh), ('dt2', hh), K('C', 'xs', 1)])
                    def cmb(e, hh=hh, hsl=hsl):
                        e.scalar_tensor_tensor(out=hbuf[:, hsl], in0=PB[EB[hh % 2]][:, 0:Dh],
                                               scalar=colsI[:, 2 * H + hh:2 * H + hh + 1], in1=hbuf[:, hsl],
                                               op0=ALU.mult, op1=ALU.add)
                        return e.scalar_tensor_tensor(out=dent[:, hh:hh + 1], in0=PB[EB[hh % 2]][:, Dh:Dh + 1],
                                                      scalar=colsI[:, 2 * H + hh:2 * H + hh + 1], in1=dt2[:, hh:hh + 1],
                                                      op0=ALU.mult, op1=ALU.add)
                    op('dve', cmb, [PSK(EB[hh % 2]), K('C', 'hbuf', hh), ('dt2', hh), CK], [K('C', 'hbuf', hh), ('dent', hh)])
                DENK = [('dent', hh) for hh in range(H)]
                op('act', lambda e: e.activation(out=rden, in_=dent, func=AF.Abs), DENK, [('rden',)])
                op('dve', lambda e: e.tensor_tensor(out=rden, in0=rden, in1=colsI[:, 3 * H:4 * H], op=ALU.max),
                   [('rden',), CK], [('rden',)])
                op('dve', lambda e: e.reciprocal(out=rden, in_=rden), [('rden',)], [('rden',)])
                for hh in range(H):
                    hsl = slice(hh * Dh, (hh + 1) * Dh)
                    op('act', lambda e, hh=hh, hsl=hsl: e.activation(out=hbuf[:, hsl], in_=hbuf[:, hsl], func=AF.Copy,
                                                                   scale=rden[:, hh:hh + 1]),
                       [K('C', 'hbuf', hh), ('rden',)], [K('C', 'hbuf', hh)])
                    op('dve', lambda e, hh=hh, hsl=hsl: e.bn_stats(out=bnst[:, hh * 6:(hh + 1) * 6], in_=hbuf[:, hsl]),
                       [K('C', 'hbuf', hh)], [('bnst', hh)])
                mv = stat[:, 48:48 + 2 * H]; rsd = stat[:, 56:56 + H]
                for hh in range(H):
                    op('dve', lambda e, hh=hh: e.bn_aggr(out=mv[:, 2 * hh:2 * hh + 2], in_=bnst[:, hh * 6:(hh + 1) * 6]),
                       [('bnst', hh)], [('mv', hh)])
                MVK = [('mv', hh) for hh in range(H)]
                op('dve', lambda e: e.tensor_scalar(out=rsd, in0=mv[:, 1:2 * H:2], scalar1=EPS, scalar2=None, op0=ALU.add),
                   MVK, [('rsd',)])
                op('act', lambda e: e.activation(out=rsd, in_=rsd, func=AF.Sqrt), [('rsd',)], [('rsd',)])
                op('dve', lambda e: e.reciprocal(out=rsd, in_=rsd), [('rsd',)], [('rsd',)])
                for hh in range(H):
                    hsl = slice(hh * Dh, (hh + 1) * Dh)
                    op('dve', lambda e, hh=hh, hsl=hsl: e.tensor_scalar(out=hbuf[:, hsl], in0=hbuf[:, hsl],
                                                                       scalar1=mv[:, 2 * hh:2 * hh + 1], scalar2=rsd[:, hh:hh + 1],
                                                                       op0=ALU.subtract, op1=ALU.mult),
                       [K('C', 'hbuf', hh), ('mv', hh), ('rsd',)], [K('C', 'hbuf', hh)])
                op('dve', lambda e: e.tensor_tensor(out=hn_store[:, ti, :], in0=hbuf, in1=ngbc, op=ALU.mult),
                   [K('C', 'hbuf', hh) for hh in range(H)] + [K('PCN', 'ng')], [K('B', 'hn', ti)])
            def ksfn(e):
                ins = None
                for hh in range(H):
                    for dc in range(DC):
                        ins = e.matmul(PB[5][:, 256 + hh * DC + dc:256 + hh * DC + dc + 1],
                                       lhsT=kw[:, i, hh, dc * 128:(dc + 1) * 128], rhs=onesb[:], start=True, stop=True)
                return ins
            op('pe', ksfn, KWK + INIT, [PSK(5)])
            op('act', lambda e: e.copy(out=ksb[:, 0:MC], in_=PB[5][:, 256:256 + MC]), [PSK(5)], [('ksb',)])
            for hh in range(H):
                bk = [7, 2][hh % 2]
                def kvfn(e, hh=hh, bk=bk):
                    ins = None
                    for dc in range(DC):
                        ins = e.matmul(PB[bk][:, dc * Dh:(dc + 1) * Dh],
                                       lhsT=kw[:, i, hh, dc * 128:(dc + 1) * 128], rhs=vsb[:, i, hh, 0:Dh], start=True, stop=True)
                    return ins
                op('pe', kvfn, KWK + VK + INIT, [PSK(bk)])
                csl_ = slice(hh * DC, (hh + 1) * DC)
                SK = K('B', 'Cst', hh); NK = ('nst', hh)
                def upd(e, hh=hh, csl_=csl_):
                    cs = Cst[:, csl_, :].rearrange("p a b -> p (a b)")
                    e.tensor_scalar(out=cs, in0=cs, scalar1=dsbI[:, hh:hh + 1], scalar2=1.0, op0=ALU.mult, op1=ALU.mult)
                    return e.tensor_scalar(out=nst[:, csl_], in0=nst[:, csl_], scalar1=dsbI[:, hh:hh + 1], scalar2=1.0,
                                           op0=ALU.mult, op1=ALU.mult)
                op('pool', upd, [SK, NK, DK] + INIT, [SK, NK])
                def upd2(e, hh=hh, csl_=csl_, bk=bk):
                    cs = Cst[:, csl_, :].rearrange("p a b -> p (a b)")
                    e.scalar_tensor_tensor(out=cs, in0=PB[bk][:, 0:DC * Dh], scalar=dsbI[:, H + hh:H + hh + 1], in1=cs,
                                           op0=ALU.mult, op1=ALU.add)
                    return e.scalar_tensor_tensor(out=nst[:, csl_], in0=ksb[:, csl_],
                                                  scalar=dsbI[:, H + hh:H + hh + 1], in1=nst[:, csl_], op0=ALU.mult, op1=ALU.add)
                op('dve', upd2, [PSK(bk), ('ksb',), SK, NK, DK], [SK, NK])

        for i in range(4):
            bk_ = s1_tile(b + 1, i, defer=True) if b + 1 < NB else None
            KOU(i)
            if bk_ is not None:
                bk_()

    for b in range(NB):
        p1_block(b)


    region_barrier('A')
    region_barrier('B')
    region_barrier('C')
    region_barrier('PCN')
    dma_sp(lng_s, lngbc, [], [K('PCN', 'ln')], 'pcn2')
    dma_sp(lnb_s, lnbbc, [], [K('PCN', 'ln')], 'pcn2')
    dma_sp(bs_s, bsbc, [], [K('PCN', 'ln')], 'pcn2')
    LNK = [K('PCN', 'ln')]

    stream = []
    def add_stream(src, kc, cbw, tag):
        stream.append(dict(src=src, kc=kc, cbw=cbw, tag=tag, loaded=None))
    nsl = max(1, GW // 512)
    for j in range(nsl): add_stream(w_in[:, c_o + j * CBW:c_o + (j + 1) * CBW], KC, CBW, ('o', j))
    for j in range(nsl): add_stream(w_in[:, c_v + j * CBW:c_v + (j + 1) * CBW], KC, CBW, ('v', j))
    for j in range(nsl): add_stream(w_in[:, c_u + j * CBW:c_u + (j + 1) * CBW], KC, CBW, ('u', j))
    DB = min(512, D)
    ABW = min(D, SL // MC)
    for j in range(D // DB):
        add_stream(w_in[:, c_ga + j * DB:c_ga + (j + 1) * DB], KC, DB, ('ga', j))
        if (j * DB) % ABW == 0:
            add_stream(w_a[:, j * DB:j * DB + ABW], MC, ABW, ('wa', (j * DB) // ABW))
    for j in range(D // DB):
        add_stream(w_in[:, c_gb + j * DB:c_gb + (j + 1) * DB], KC, DB, ('gb', j))
        if (j * DB) % ABW == 0:
            add_stream(w_b[:, j * DB:j * DB + ABW], MC, ABW, ('wb', (j * DB) // ABW))
    for hf in range(2):
        for j in range(D // DB): add_stream(w_out[:, j * DB:(j + 1) * DB], KC, DB, ('wo', hf, j))
        for gq in range(NG):
            for j in range(FG // DB): add_stream(w_ff1[:, gq * FG + j * DB:gq * FG + (j + 1) * DB], KC, DB, ('f1', hf, gq, j))
            for j in range(D // DB): add_stream(w_ff2[gq * FG:(gq + 1) * FG, j * DB:(j + 1) * DB], KF, DB, ('f2', hf, gq, j))
    spos = dict(issued=0)
    sidx = {s['tag']: n for n, s in enumerate(stream)}

    def get_slab(tag, look=2):
        n = sidx[tag]
        upto = min(len(stream), n + 1 + look)
        while spos['issued'] < upto:
            s = stream[spos['issued']]
            s['loaded'] = slab_load(s['src'], s['kc'], s['cbw'])
            spos['issued'] += 1
        v_, k_, g_ = stream[n]['loaded']
        assert ring_state['n'] <= g_ + 4, ("slab overwritten before use", tag)
        return v_, k_

    get_slab(('o', 0), look=1)
    stg_bufs = [(C_f[:, 6144:6144 + D], K('C', 'stg', 0), 'xst0'),
                (B_f[:, (NT * GW + 2 * D) // 2:(NT * GW + 2 * D) // 2 + D], K('B', 'stg', 1), 'xst1')]
    for t in range(NT):
        xi = xdma['n'] % 2; xdma['n'] += 1
        r0 = NPREV + t * 128
        xkey = K('A', 'xst2', xi)
        stg, stg_key, stg_sem = stg_bufs[xi]
        dma_sp(stg, xseq[r0:r0 + 128, :], [], [stg_key], stg_sem)
        xs[0] = B[:, NT * GW:NT * GW + D]
        xs[1] = B[:, NT * GW + D:NT * GW + 2 * D]
        norm_transpose(stg, stg_key, g1s, xnT, t * 128, [K('A', 'xnT', t)], xi, 2 * xi, xreg='B', use_pool=True)
    XNK = [K('A', 'xnT', t) for t in range(NT)]
    XSK = []
    XSB = [K('B', 'xs', 0), K('B', 'xs', 1), K('B', 'stg', 1)]

    def gelu_from_psum(bank_ap, n, par, out_ap, out_keys, extra_reads=()):
        x_ = gx[par][:, 0:n]; t_ = gt[par][:, 0:n]; z_ = gz[par][:, 0:n]
        kx = K('C', 'gx', par); kt = K('C', 'gt', par); kz = K('C', 'gz', par)
        op('act', lambda e: e.copy(out=x_, in_=bank_ap), list(extra_reads), [kx])
        op('dve', lambda e: e.scalar_tensor_tensor(out=t_, in0=x_, scalar=0.044715, in1=x_, op0=ALU.mult, op1=ALU.mult),
           [kx], [kt])
        op('dve', lambda e: e.scalar_tensor_tensor(out=z_, in0=t_, scalar=1.0, in1=x_, op0=ALU.add, op1=ALU.mult),
           [kt, kx], [kz])
        op('act', lambda e: e.activation(out=z_, in_=z_, func=AF.Sigmoid, scale=1.5957691216057308), [kz], [kz])
        op('dve', lambda e: e.tensor_tensor(out=out_ap, in0=z_, in1=x_, op=ALU.mult), [kz, kx], out_keys)

    pbank = dict(n=0)
    def nextbank():
        b = pbank['n'] % 8
        pbank['n'] += 1
        return b

    o_pend = [None]
    for t in range(NT):
        for j in range(nsl):
            sl, skey = get_slab(('o', j))
            bk = nextbank()
            mm_tm(PB[bk][:, 0:CBW], xnT, t * 128, sl, 0, CBW, KC, [K('A', 'xnT', t), skey], [PSK(bk)])
            par = (t * nsl + j) % 2
            ytm = [ybtm, gt[1].bitcast(BF16)[:, 0:512]][par]; yk = K('C', 'ybtm', par)
            op('act', lambda e, bk=bk, par=par: e.activation(out=gx[par][:, 0:CBW], in_=PB[bk][:, 0:CBW], func=AF.Sigmoid),
               [PSK(bk)], [K('C', 'gx', par)])
            op('dve', lambda e, j=j, t=t, par=par, ytm=ytm: e.tensor_tensor(out=ytm[:, 0:CBW], in0=gx[par][:, 0:CBW],
                                                          in1=hn_store[:, t, j * CBW:(j + 1) * CBW], op=ALU.mult),
               [K('C', 'gx', par), K('B', 'hn', t)], [yk, K('C', 'gt', 1)])
            bk2 = nextbank()
            def o_back(bk2=bk2, j=j, t=t, ytm=ytm, yk=yk):
                def trf(e):
                    ins = None
                    for c in range(CPS):
                        ins = e.transpose(out=pbf(bk2)[:, c * 128:(c + 1) * 128], in_=ytm[:, c * 128:(c + 1) * 128],
                                          identity=identb[:])
                    return ins
                op('pe', trf, [yk] + INIT, [PSK(bk2)])
                op('act', lambda e: e.copy(
                    out=ybT[:, j * CPS:(j + 1) * CPS, t * 128:(t + 1) * 128],
                    in_=pbf(bk2)[:, 0:CPS * 128].rearrange("p (c t) -> p c t", c=CPS)),
                   [PSK(bk2)] + XSK, [K('A', 'ybT', t)])
            if o_pend[0] is not None:
                o_pend[0]()
            o_pend[0] = o_back
    if o_pend[0] is not None:
        o_pend[0]()
    YBK = [K('A', 'ybT', t) for t in range(NT)]
    for t in range(NT):
        for j in range(nsl):
            sl, skey = get_slab(('v', j))
            bk = nextbank()
            mm_tm(PB[bk][:, 0:CBW], xnT, t * 128, sl, 0, CBW, KC, [K('A', 'xnT', t), skey], [PSK(bk)])
            gelu_from_psum(PB[bk][:, 0:CBW], CBW, j % 2, gv[:, j * CBW:(j + 1) * CBW], [K('C', 'gv', j)], [PSK(bk)])
        GVK = [K('C', 'gv', j) for j in range(nsl)]
        def bnf(e):
            ins = None
            for j in range(nsl):
                ins = e.bn_stats(out=bnst[:, j * 6:(j + 1) * 6], in_=gv[:, j * CBW:(j + 1) * CBW])
            return ins
        op('dve', bnf, GVK, [('bnst', 0)])
        op('dve', lambda e: e.bn_aggr(out=bnst[:, 12:14], in_=bnst[:, 0:6 * nsl]), [('bnst', 0)], [('bnst', 1)])
        def rs3(e):
            return e.tensor_scalar(out=bnst[:, 14:15], in0=bnst[:, 13:14], scalar1=EPS, scalar2=None, op0=ALU.add)
        op('dve', rs3, [('bnst', 1)], [('bnst', 2)])
        op('act', lambda e: e.activation(out=bnst[:, 14:15], in_=bnst[:, 14:15], func=AF.Sqrt), [('bnst', 2)], [('bnst', 2)])
        op('dve', lambda e: e.reciprocal(out=bnst[:, 14:15], in_=bnst[:, 14:15]), [('bnst', 2)], [('bnst', 2)])
        op('dve', lambda e: e.scalar_tensor_tensor(out=tm1, in0=gv, scalar=bnst[:, 12:13], in1=lng_s, op0=ALU.subtract,
                                                   op1=ALU.mult), GVK + [('bnst', 1)] + LNK, [K('C', 'tm1')])
        op('dve', lambda e, t=t: e.scalar_tensor_tensor(out=v_ln[:, t, :], in0=tm1, scalar=bnst[:, 14:15], in1=lnb_s,
                                                        op0=ALU.mult, op1=ALU.add),
           [K('C', 'tm1'), ('bnst', 2)] + LNK, [K('B', 'vln', t)] + XSB)
    for j in range(nsl):
        sl, skey = get_slab(('u', j))
        for c in range(CPS):
            for n in range(NBO):
                bk = nextbank()
                mm_fm(PB[bk][:, 0:BLK], sl, c * 128, xnT, n * BLK, BLK, KC, XNK + [skey], [PSK(bk)])
                gelu_from_psum(PB[bk][:, 0:BLK], BLK, (c + n) % 2, yaT[:, j * CPS + c, n * BLK:(n + 1) * BLK],
                               [K('A', 'yaT', j * CPS + c, n)], [PSK(bk)])
    for t in range(NT):
        nbk = (G * 128 + 511) // 512
        bks = [nextbank() for _ in range(nbk)]
        def spf(e, t=t, bks=bks):
            ins = None
            for g in range(G):
                ins = e.matmul(PB[bks[g // 4]][:, (g % 4) * 128:(g % 4 + 1) * 128], lhsT=v_ln[:, t, g * 128:(g + 1) * 128],
                               rhs=wsTm[:, g * 128:(g + 1) * 128], start=True, stop=True)
            return ins
        op('pe', spf, [K('B', 'vln', t)] + INIT, [PSK(b_) for b_ in bks])
        for q, bk in enumerate(bks):
            g0 = q * 4; g1 = min(G, g0 + 4); w_ = (g1 - g0) * 128
            op('dve', lambda e, bk=bk, g0=g0, w_=w_: e.tensor_tensor(out=tm1[:, 0:w_], in0=PB[bk][:, 0:w_],
                                                                    in1=bs_s[:, g0 * 128:g0 * 128 + w_], op=ALU.add),
               [PSK(bk)] + LNK, [K('C', 'tm1')])
            rk = [K('A', 'yaT', g, (t * 128) // BLK) for g in range(g0, g1)]
            op('dve', lambda e, g0=g0, g1=g1, t=t: e.tensor_tensor(
                out=yaT[:, g0:g1, t * 128:(t + 1) * 128], in0=tm1[:, 0:(g1 - g0) * 128].rearrange("p (g t) -> p g t", g=g1 - g0),
                in1=yaT[:, g0:g1, t * 128:(t + 1) * 128], op=ALU.mult),
               [K('C', 'tm1')] + rk, rk)
    YAK = [K('A', 'yaT', g, n) for g in range(G) for n in range(NBO)]

    region_barrier('B')
    for (gtag, wtag, yT, ykeys, gi, first) in (('ga', 'wa', yaT, YAK, 0, True), ('gb', 'wb', ybT, YBK, 1, False)):
        for j in range(KC):
            slg, kg = get_slab((gtag, (j * 128) // DB))
            slw, kw_ = get_slab((wtag, (j * 128) // ABW))
            for n in range(NBO):
                b1 = nextbank(); b2 = nextbank()
                mm_fm(PB[b1][:, 0:BLK], slg, (j * 128) % DB, xnT, n * BLK, BLK, KC, XNK + [kg], [PSK(b1)])
                mm_fm(PB[b2][:, 0:BLK], slw, (j * 128) % ABW, yT, n * BLK, BLK, MC, ykeys + [kw_], [PSK(b2)])
                par = (j + n) % 2
                op('act', lambda e, b1=b1, j=j, par=par, gi=gi: e.activation(
                    out=gx[par], in_=PB[b1][:, 0:BLK], func=AF.Sigmoid, bias=bgate[:, gi * KC + j:gi * KC + j + 1]),
                   [PSK(b1)] + INIT, [K('C', 'gx', par)])
                mk = K('B', 'mixedT', j, n)
                if first:
                    op('dve', lambda e, b2=b2, j=j, n=n, par=par: e.tensor_tensor(
                        out=mixedT[:, j, n * BLK:(n + 1) * BLK], in0=gx[par], in1=PB[b2][:, 0:BLK], op=ALU.mult),
                       [K('C', 'gx', par), PSK(b2)], [mk])
                else:
                    op('dve', lambda e, b2=b2, par=par: e.tensor_tensor(out=gt[par], in0=gx[par], in1=PB[b2][:, 0:BLK],
                                                                        op=ALU.mult),
                       [K('C', 'gx', par), PSK(b2)], [K('C', 'gt', par)])
                    op('dve', lambda e, j=j, n=n, par=par: e.tensor_tensor(
                        out=mixedT[:, j, n * BLK:(n + 1) * BLK], in0=mixedT[:, j, n * BLK:(n + 1) * BLK], in1=gt[par],
                        op=ALU.add), [K('C', 'gt', par), mk], [mk])
    MXK = [K('B', 'mixedT', j, n) for j in range(KC) for n in range(NBO)]

    for hf in range(2):
        region_barrier('A')
        if hf == 1:
            region_barrier('C')
        tok0 = NPREV + hf * HT
        for t in range(NHT):
            dma_sp(x1[:, t, :], xseq[tok0 + t * 128:tok0 + (t + 1) * 128, :], [], [K('A', 'x1', t)], 'x1_%d' % t)
        for j in range(D // DB):
            sl, skey = get_slab(('wo', hf, j))
            for t in range(NHT):
                bk = nextbank()
                mm_tm(PB[bk][:, 0:DB], mixedT, hf * HT + t * 128, sl, 0, DB, KC, MXK + [skey], [PSK(bk)])
                op('dve', lambda e, bk=bk, t=t, j=j: e.tensor_tensor(out=x1[:, t, j * DB:(j + 1) * DB],
                                                                      in0=x1[:, t, j * DB:(j + 1) * DB], in1=PB[bk][:, 0:DB],
                                                                      op=ALU.add),
                   [PSK(bk), K('A', 'x1', t)], [K('A', 'x1', t)])
        xs[0] = C[:, 8192:8192 + D]; xs[1] = C[:, 8192 + D:8192 + 2 * D]
        for t in range(NHT):
            norm_transpose(x1[:, t, :], K('A', 'x1', t), g2s, hnT, t * 128, [K('A', 'hnT', t)], t % 2, 2 * (t % 2), xreg='C')
        HNK = [K('A', 'hnT', t) for t in range(NHT)]
        for gq in range(NG):
            for j in range(FG // DB):
                sl, skey = get_slab(('f1', hf, gq, j))
                for c in range(DB // 128):
                    for n in range(HT // FB):
                        bk = nextbank()
                        mm_fm(PB[bk][:, 0:FB], sl, c * 128, hnT, n * FB, FB, KC, HNK + [skey], [PSK(bk)])
                        par = (c + n) % 2
                        op('act', lambda e, bk=bk, par=par: e.activation(out=gx[par][:, 0:FB], in_=PB[bk][:, 0:FB], func=AF.Relu),
                           [PSK(bk)], [K('C', 'gx', par)])
                        kc_ = j * (DB // 128) + c
                        op('dve', lambda e, par=par, kc_=kc_, n=n: e.tensor_tensor(
                            out=h1T[:, kc_, n * FB:(n + 1) * FB], in0=gx[par][:, 0:FB], in1=gx[par][:, 0:FB], op=ALU.mult),
                           [K('C', 'gx', par)], [K('A', 'h1T', kc_, n)])
            H1K = [K('A', 'h1T', k_, n) for k_ in range(KF) for n in range(HT // FB)]
            for j in range(D // DB):
                sl, skey = get_slab(('f2', hf, gq, j))
                for t in range(NHT):
                    bk = nextbank()
                    mm_tm(PB[bk][:, 0:DB], h1T, t * 128, sl, 0, DB, KF, H1K + [skey], [PSK(bk)])
                    op('dve', lambda e, bk=bk, t=t, j=j: e.tensor_tensor(out=x1[:, t, j * DB:(j + 1) * DB],
                                                                          in0=x1[:, t, j * DB:(j + 1) * DB],
                                                                          in1=PB[bk][:, 0:DB], op=ALU.add),
                       [PSK(bk), K('A', 'x1', t)], [K('A', 'x1', t)])
        region_barrier('C')
        dma_sp(gfb, gfbc, [], [K('C', 'gfb')], 'gfb')
        for t in range(NHT):
            ss = stat[:, 16:17]; rs = stat[:, 17:18]
            op('act', lambda e, t=t: e.activation(out=junkF, in_=x1[:, t, :], func=AF.Square, accum_out=ss),
               [K('A', 'x1', t)], [K('C', 'junkF'), ('stat', 16)])
            def rsf(e):
                return e.tensor_scalar(out=rs, in0=ss, scalar1=1.0 / D, scalar2=EPS, op0=ALU.mult, op1=ALU.add)
            op('dve', rsf, [('stat', 16)], [('stat', 17)])
            op('act', lambda e: e.activation(out=rs, in_=rs, func=AF.Sqrt), [('stat', 17)], [('stat', 17)])
            op('dve', lambda e: e.reciprocal(out=rs, in_=rs), [('stat', 17)], [('stat', 17)])
            oi = t % 2
            op('dve', lambda e, t=t, oi=oi: e.scalar_tensor_tensor(out=ost[oi], in0=x1[:, t, :], scalar=rs, in1=gfb,
                                                                   op0=ALU.mult, op1=ALU.mult),
               [K('A', 'x1', t), ('stat', 17), K('C', 'gfb')], [K('C', 'ost', oi)])
            r0 = hf * HT + t * 128
            dma_sp(out_d[r0:r0 + 128, :], ost[oi], [K('C', 'ost', oi)], [('outd', hf, t)], 'ost%d' % oi)
    S.op('sp', None, reads=[('outd', hf, t) for hf in range(2) for t in range(NHT)], writes=[])

    dnames = S.finalize()
    sems = {}
    for e_ in ('pe', 'act', 'dve', 'pool', 'sp'):
        sems[('eng', e_)] = es.enter_context(nc.semaphore("s_" + e_))
    for dn_ in dnames:
        sems[('dma', dn_)] = es.enter_context(nc.semaphore("d_" + dn_))
    with nc.Block() as block:
        @block.tensor
        def _(e):
            S.emit_engine('pe', e, sems)

        @block.scalar
        def _(e):
            S.emit_engine('act', e, sems)

        @block.vector
        def _(e):
            S.emit_engine('dve', e, sems)

        @block.gpsimd
        def _(e):
            S.emit_engine('pool', e, sems)

        @block.sync
        def _(e):
            S.emit_engine('sp', e, sems)
    es.close()
    return nc


def make_inputs(cfg, p, core):
    D = cfg['D']; T = cfg['T']; NPREV = cfg['NPREV']; H = cfg['H']
    GW = D // 2; G = GW // 128; Dh = GW // H; DC = Dh // 128; KC = D // 128; MC = GW // 128
    NCHT = (NPREV + T) // 128
    x = p['x']
    Bn, Sn, _ = x.shape
    cps = Sn // T
    b = core // cps; pos = core % cps
    xs_ = x[b]
    nreal = pos * T
    xseq = np.zeros((NPREV + T, D), np.float32)
    if nreal:
        xseq[NPREV - nreal:NPREV] = xs_[0:nreal]
    xseq[NPREV:] = xs_[nreal:nreal + T]
    mask = np.ones((H, NCHT), np.float32)
    mask[:, 0:(NPREV - nreal) // 128] = 0.0
    f32 = lambda a: np.ascontiguousarray(a, dtype=np.float32)
    fm = lambda v: f32(v.reshape(-1, 128).T)
    bc = lambda v: f32(np.broadcast_to(v.reshape(1, -1), (128, v.size)))
    tri = np.triu(np.ones((128, 128), np.float32))
    sel = np.zeros((H, H * 128), np.float32)
    for h in range(H):
        sel[h, h * 128:(h + 1) * 128] = 1.0
    m = {
        "xseq": xseq,
        "w_in": f32(p['w_in'][0]), "w_a": f32(p['w_a'][0]), "w_b": f32(p['w_b'][0]), "w_out": f32(p['w_out'][0]),
        "w_ff1": f32(p['w_ff1'][0]), "w_ff2": f32(p['w_ff2'][0]),
        "g1fm": fm(p['norm1_g'][0]), "g2fm": fm(p['norm2_g'][0]), "gfbc": bc(p['norm_f_g']),
        "lngbc": bc(p['gm_ln_g'][0]), "lnbbc": bc(p['gm_ln_b'][0]), "bsbc": bc(p['gm_bs'][0].reshape(-1)),
        "wsT": f32(np.transpose(p['gm_ws'][0], (2, 0, 1)).reshape(128, G * 128)),
        "tri": tri,
        "cw": f32(np.transpose(p['ml_conv_w'][0].reshape(4, MC, 128), (2, 1, 0)).reshape(128, MC * 4)),
        "cb": fm(p['ml_conv_b'][0]),
        "wq_s": f32(np.transpose(p['ml_wq'][0].reshape(H, DC, 128, Dh), (2, 0, 1, 3)).reshape(128, MC * Dh)),
        "wk_s": f32(np.transpose(p['ml_wk'][0].reshape(H, DC, 128, Dh), (2, 0, 1, 3)).reshape(128, MC * Dh)),
        "igb": f32(p['ml_ig_b'][0].reshape(H, 1)), "fgb": f32(p['ml_fg_b'][0].reshape(H, 1)),
        "ngbc": bc(p['ml_norm_g'][0]),
        "bgate": f32(np.transpose(p['b_gate'][0].reshape(2, KC, 128), (2, 0, 1)).reshape(128, 2 * KC)),
        "mask": mask, "ident": np.eye(128, dtype=np.float32), "sel": sel,
    }
    return m


_NC_CACHE = {}


def kernel(**inputs):
    cfg = REAL_CFG
    p = {k: np.asarray(v) for k, v in inputs.items()}
    n = cfg['NCORES']
    key = tuple(sorted(cfg.items()))
    if key not in _NC_CACHE:
        _NC_CACHE[key] = build(cfg)
    nc = _NC_CACHE[key]
    in_maps = [make_inputs(cfg, p, c) for c in range(n)]
    res = run_bass_kernel_spmd(nc, in_maps, core_ids=list(range(n)))
    Bn, Sn, D = p['x'].shape
    outs = [np.asarray(r["out"], dtype=np.float32) for r in res.results]
    return np.concatenate(outs, axis=0).reshape(Bn, Sn, D)
```
